# Optimizing a Trainium2 kernel written in Bass

```python
import math
import jax, jax.numpy as jnp
from jax import lax
import numpy as np

D_MODEL = 4096
BATCH = 4
SEQ = 2048
DEPTH = 2

HEAD_DIM = 128
MIX_WIDTH = D_MODEL
MOBA_HEADS = (MIX_WIDTH // 2) // HEAD_DIM
MOBA_WIDTH = MOBA_HEADS * HEAD_DIM
MOBA_BLOCK = 256
MOBA_TOPK = 3
MOBA_Q_CHUNK = 64
DIFF_QK_DIM = 128
DIFF_V_DIM = 2 * DIFF_QK_DIM
DIFF_HEADS = (MIX_WIDTH // 2) // DIFF_V_DIM
DIFF_QK_WIDTH = DIFF_HEADS * 2 * DIFF_QK_DIM
DIFF_V_WIDTH = DIFF_HEADS * DIFF_V_DIM
DIFF_Q_BLOCK = 128
IN_WIDTH = 3 * MOBA_WIDTH + 2 * DIFF_QK_WIDTH + DIFF_V_WIDTH + 2 * D_MODEL
D_FF = 4 * D_MODEL
EPS = 1e-6
NEG = -1e30

kernel_name = "hybrid_moba_diffattn_gated_block"


def rms_norm(x, g):
    xf = x.astype(jnp.float32)
    y = xf * lax.rsqrt(jnp.mean(xf * xf, axis=-1, keepdims=True) + EPS)
    return (y * g.astype(jnp.float32)).astype(x.dtype)


def alibi_slopes(n_heads):
    return 2.0 ** (-8.0 * jnp.arange(1, n_heads + 1, dtype=jnp.float32) / n_heads)


def moba_attention(q, k, v):
    B, H, S, Dh = q.shape
    nb = -(-S // MOBA_BLOCK)
    s_pad = nb * MOBA_BLOCK
    pad = s_pad - S
    kp = jnp.pad(k, ((0, 0), (0, 0), (0, pad), (0, 0)))
    vp = jnp.pad(v, ((0, 0), (0, 0), (0, pad), (0, 0)))
    kb = kp.reshape(B, H, nb, MOBA_BLOCK, Dh)
    vb = vp.reshape(B, H, nb, MOBA_BLOCK, Dh)
    k_mean = jnp.mean(kb.astype(jnp.float32), axis=3)
    topk = min(MOBA_TOPK, nb)
    scale = Dh ** -0.5
    slopes = alibi_slopes(H)
    sl5 = slopes[None, :, None, None, None]
    sl4 = slopes[None, :, None, None]
    bi = jnp.arange(B)[:, None, None, None]
    hi = jnp.arange(H)[None, :, None, None]
    n_chunks = S // MOBA_Q_CHUNK
    blk_ar = jnp.arange(MOBA_BLOCK)

    def chunk(c):
        t0 = c * MOBA_Q_CHUNK
        qc = lax.dynamic_slice_in_dim(q, t0, MOBA_Q_CHUNK, axis=2)
        pos_q = t0 + jnp.arange(MOBA_Q_CHUNK)
        own = t0 // MOBA_BLOCK
        gate = jnp.einsum('bhcd,bhnd->bhcn', qc.astype(jnp.float32), k_mean)
        past = jnp.arange(nb) < own
        gate = jnp.where(past, gate, NEG)
        _, idx = lax.top_k(gate, topk)
        valid = idx < own
        k_sel = kb[bi, hi, idx]
        v_sel = vb[bi, hi, idx]
        s_sel = jnp.einsum('bhcd,bhcjsd->bhcjs', qc, k_sel).astype(jnp.float32) * scale
        pos_sel = idx[..., None] * MOBA_BLOCK + blk_ar
        s_sel = s_sel - sl5 * (pos_q[:, None, None] - pos_sel).astype(jnp.float32)
        s_sel = jnp.where(valid[..., None], s_sel, NEG)
        k_own = lax.dynamic_slice_in_dim(kp, own * MOBA_BLOCK, MOBA_BLOCK, axis=2)
        v_own = lax.dynamic_slice_in_dim(vp, own * MOBA_BLOCK, MOBA_BLOCK, axis=2)
        s_own = jnp.einsum('bhcd,bhsd->bhcs', qc, k_own).astype(jnp.float32) * scale
        dist = (pos_q[:, None] - (own * MOBA_BLOCK + blk_ar)[None, :]).astype(jnp.float32)
        s_own = jnp.where(dist >= 0, s_own - sl4 * dist, NEG)
        scores = jnp.concatenate(
            [s_sel.reshape(B, H, MOBA_Q_CHUNK, topk * MOBA_BLOCK), s_own], axis=-1)
        p = jax.nn.softmax(scores, axis=-1)
        p_sel = p[..., :topk * MOBA_BLOCK].reshape(B, H, MOBA_Q_CHUNK, topk, MOBA_BLOCK).astype(v.dtype)
        p_own = p[..., topk * MOBA_BLOCK:].astype(v.dtype)
        return (jnp.einsum('bhcjs,bhcjsd->bhcd', p_sel, v_sel)
                + jnp.einsum('bhcs,bhsd->bhcd', p_own, v_own))

    outs = lax.map(chunk, jnp.arange(n_chunks))
    return outs.transpose(1, 2, 0, 3, 4).reshape(B, H, S, Dh)


def diff_attention(q, k, v, lam, lam_init, g_subln):
    B, H, _, S, dq = q.shape
    dv = v.shape[-1]
    scale = dq ** -0.5
    sl = alibi_slopes(H)[None, :, None, None, None]
    pos_k = jnp.arange(S)
    n_blocks = S // DIFF_Q_BLOCK

    def block(i):
        t0 = i * DIFF_Q_BLOCK
        qb = lax.dynamic_slice_in_dim(q, t0, DIFF_Q_BLOCK, axis=3)
        s = jnp.einsum('bhmqd,bhmsd->bhmqs', qb, k).astype(jnp.float32) * scale
        dist = ((t0 + jnp.arange(DIFF_Q_BLOCK))[:, None] - pos_k[None, :]).astype(jnp.float32)
        s = jnp.where(dist >= 0, s - sl * dist, NEG)
        p = jax.nn.softmax(s, axis=-1)
        a = p[:, :, 0] - lam * p[:, :, 1]
        return jnp.einsum('bhqs,bhsd->bhqd', a.astype(v.dtype), v)

    o = lax.map(block, jnp.arange(n_blocks))
    o = o.transpose(1, 2, 0, 3, 4).reshape(B, H, S, dv)
    return rms_norm(o, g_subln) * (1.0 - lam_init)


def hybrid_layer(x, layer_idx, w_in, w_proj_a, w_proj_b, w_out, w_up, w_down,
                 g_pre_mix, g_post_mix, g_pre_mlp, g_post_mlp, g_subln,
                 lam_q1, lam_k1, lam_q2, lam_k2):
    B, S, _ = x.shape
    h = rms_norm(x, g_pre_mix)
    proj = jnp.einsum('bsd,de->bse', h, w_in)
    offs = np.cumsum([MOBA_WIDTH, MOBA_WIDTH, MOBA_WIDTH, DIFF_QK_WIDTH,
                      DIFF_QK_WIDTH, DIFF_V_WIDTH, D_MODEL]).tolist()
    qa, ka, va, qb, kb, vb, ga, gb = jnp.split(proj, offs, axis=-1)

    def heads(t, n, d):
        return t.reshape(B, S, n, d).transpose(0, 2, 1, 3)

    o_a = moba_attention(heads(qa, MOBA_HEADS, HEAD_DIM), heads(ka, MOBA_HEADS, HEAD_DIM),
                         heads(va, MOBA_HEADS, HEAD_DIM))
    o_a = o_a.transpose(0, 2, 1, 3).reshape(B, S, MOBA_WIDTH)

    lam_init = 0.8 - 0.6 * math.exp(-0.3 * layer_idx)
    lam = (jnp.exp(jnp.sum(lam_q1.astype(jnp.float32) * lam_k1.astype(jnp.float32)))
           - jnp.exp(jnp.sum(lam_q2.astype(jnp.float32) * lam_k2.astype(jnp.float32)))
           + lam_init)
    qd = qb.reshape(B, S, DIFF_HEADS, 2, DIFF_QK_DIM).transpose(0, 2, 3, 1, 4)
    kd = kb.reshape(B, S, DIFF_HEADS, 2, DIFF_QK_DIM).transpose(0, 2, 3, 1, 4)
    vd = heads(vb, DIFF_HEADS, DIFF_V_DIM)
    o_b = diff_attention(qd, kd, vd, lam, lam_init, g_subln)
    o_b = o_b.transpose(0, 2, 1, 3).reshape(B, S, DIFF_V_WIDTH)

    y = (jax.nn.sigmoid(ga) * jnp.einsum('bse,ed->bsd', o_a, w_proj_a)
         + jax.nn.sigmoid(gb) * jnp.einsum('bse,ed->bsd', o_b, w_proj_b))
    mix = jnp.einsum('bsd,de->bse', y, w_out)
    x = x + rms_norm(mix, g_post_mix)
    h = rms_norm(x, g_pre_mlp)
    u = jax.nn.relu(jnp.einsum('bsd,df->bsf', h, w_up))
    m = jnp.einsum('bsf,fd->bsd', u * u, w_down)
    return x + rms_norm(m, g_post_mlp)


def setup_inputs(seed: int = 0) -> dict:
    key = jax.random.key(seed)
    ks = jax.random.split(key, 17)
    f32 = jnp.float32

    def nrm(k, shape, fan_in):
        return jax.random.normal(k, shape, f32) * (fan_in ** -0.5)

    def gain(k, shape):
        return 1.0 + 0.05 * jax.random.normal(k, shape, f32)

    return {
        "x": jax.random.normal(ks[0], (BATCH, SEQ, D_MODEL), f32),
        "w_in": nrm(ks[1], (DEPTH, D_MODEL, IN_WIDTH), D_MODEL),
        "w_proj_a": nrm(ks[2], (DEPTH, MOBA_WIDTH, D_MODEL), MOBA_WIDTH),
        "w_proj_b": nrm(ks[3], (DEPTH, DIFF_V_WIDTH, D_MODEL), DIFF_V_WIDTH),
        "w_out": nrm(ks[4], (DEPTH, D_MODEL, D_MODEL), D_MODEL),
        "w_up": nrm(ks[5], (DEPTH, D_MODEL, D_FF), D_MODEL),
        "w_down": nrm(ks[6], (DEPTH, D_FF, D_MODEL), D_FF),
        "g_pre_mix": gain(ks[7], (DEPTH, D_MODEL)),
        "g_post_mix": gain(ks[8], (DEPTH, D_MODEL)),
        "g_pre_mlp": gain(ks[9], (DEPTH, D_MODEL)),
        "g_post_mlp": gain(ks[10], (DEPTH, D_MODEL)),
        "g_subln": gain(ks[11], (DEPTH, DIFF_V_DIM)),
        "lam_q1": 0.1 * jax.random.normal(ks[12], (DEPTH, DIFF_QK_DIM), f32),
        "lam_k1": 0.1 * jax.random.normal(ks[13], (DEPTH, DIFF_QK_DIM), f32),
        "lam_q2": 0.1 * jax.random.normal(ks[14], (DEPTH, DIFF_QK_DIM), f32),
        "lam_k2": 0.1 * jax.random.normal(ks[15], (DEPTH, DIFF_QK_DIM), f32),
    }


def reference(x, w_in, w_proj_a, w_proj_b, w_out, w_up, w_down,
              g_pre_mix, g_post_mix, g_pre_mlp, g_post_mlp, g_subln,
              lam_q1, lam_k1, lam_q2, lam_k2):
    for l in range(DEPTH):
        x = hybrid_layer(x, l, w_in[l], w_proj_a[l], w_proj_b[l], w_out[l], w_up[l], w_down[l],
                         g_pre_mix[l], g_post_mix[l], g_pre_mlp[l], g_post_mlp[l], g_subln[l],
                         lam_q1[l], lam_k1[l], lam_q2[l], lam_k2[l])
    return x
```

```python
import contextlib
import math
import numpy as np
import ml_dtypes
import concourse.bass as bass
import concourse.mybir as mybir
from concourse.bass_utils import run_bass_kernel_spmd

F32 = mybir.dt.float32
BF16 = mybir.dt.bfloat16
ALU = mybir.AluOpType
AF = mybir.ActivationFunctionType
AX = mybir.AxisListType

D = 4096
NC_ = 32
T = 1024
DEPTH = 2
INW = 20480
DFF = 16384
EPS = 1e-6
SCALE = 128.0 ** -0.5
NEGBIG = -1.0e9
SELBIG = 30000.0
ENGINES = ["sync", "scalar", "gpsimd", "vector", "tensor"]
PIECES = {
    "winA": (4096, 8192),
    "winB": (4096, 12288),
    "wpa": (2048, 4096), "wpb": (2048, 4096), "wout": (4096, 4096),
    "wupA": (4096, 8192), "wupB": (4096, 8192),
    "wdnA": (8192, 4096), "wdnB": (8192, 4096),
}
NT0 = 12
NT1 = 16


class Ev:
    __slots__ = ("sem", "val")

    def __init__(self, sem, val):
        self.sem = sem
        self.val = val


class Sched:
    def __init__(self, nc):
        self.nc = nc
        self.q = {e: [] for e in ENGINES}
        self.sems = {}
        self.waited = {e: {} for e in ENGINES}
        self._ctx = []
        self.last = {}

    def sem(self, key):
        if key not in self.sems:
            cm = self.nc.semaphore("s_" + key)
            h = cm.__enter__()
            self._ctx.append(cm)
            self.sems[key] = [h, 0]
        return self.sems[key]

    def _waits(self, eng, waits):
        best = {}
        for ev in waits:
            if ev is None:
                continue
            if best.get(ev.sem, -1) < ev.val:
                best[ev.sem] = ev.val
        out = []
        w = self.waited[eng]
        for k, v in best.items():
            if w.get(k, -1) >= v:
                continue
            w[k] = v
            out.append(Ev(k, v))
        return out

    def op(self, eng, fn, waits=(), sig=True):
        ws = self._waits(eng, waits)
        ev = None
        if sig:
            s = self.sem("E_" + eng)
            s[1] += 1
            ev = Ev("E_" + eng, s[1])
            self.last[eng] = ev
        self.q[eng].append((fn, ws, ev, 1))
        return ev

    def dma(self, eng, fn, key, waits=(), inc=16):
        ws = self._waits(eng, waits)
        s = self.sem("D_" + key)
        s[1] += inc
        ev = Ev("D_" + key, s[1])
        self.q[eng].append((fn, ws, ev, inc))
        return ev

    def wait_only(self, eng, waits):
        ws = self._waits(eng, waits)
        if ws:
            self.q[eng].append((None, ws, None, 0))

    def all_events(self):
        evs = [ev for ev in self.last.values()]
        for k, v in self.sems.items():
            if k.startswith("D_") and v[1] > 0:
                evs.append(Ev(k, v[1]))
        return evs

    def barrier(self):
        evs = self.all_events()
        for e in ENGINES:
            self.wait_only(e, evs)

    def emit(self):
        nc = self.nc
        sems = self.sems
        with nc.Block() as block:
            def mk(engname):
                def body(e):
                    for fn, ws, ev, inc in self.q[engname]:
                        for w in ws:
                            e.wait_ge(sems[w.sem][0], w.val)
                        if fn is None:
                            continue
                        ins = fn(e)
                        if ev is not None:
                            if ev.sem.startswith("D_cc"):
                                ins.then_inc(sems[ev.sem][0])
                            else:
                                ins.then_inc(sems[ev.sem][0], inc)
                return body
            block.sync(mk("sync"))
            block.scalar(mk("scalar"))
            block.gpsimd(mk("gpsimd"))
            block.vector(mk("vector"))
            block.tensor(mk("tensor"))

    def close(self):
        for cm in reversed(self._ctx):
            cm.__exit__(None, None, None)


class WStream:
    def __init__(self, s, name, slots, tasks, lookahead, eng="sync", deps=None):
        self.s = s
        self.name = name
        self.slots = slots
        self.tasks = tasks
        self.look = min(lookahead, len(slots) - 1)
        self.emitted = 0
        self.loaded = {}
        self.free = [None] * len(slots)
        self.eng = eng
        self.deps = deps or (lambda j: [])

    def ensure(self, i):
        upto = min(len(self.tasks), i + self.look + 1)
        while self.emitted < upto:
            j = self.emitted
            sl = j % len(self.slots)
            fns = self.tasks[j](self.slots[sl])
            if not isinstance(fns, (list, tuple)):
                fns = [fns]
            for fn in fns:
                self.loaded[j] = self.s.dma(self.eng, fn, f"{self.name}{sl}", waits=[self.free[sl]] + list(self.deps(j)))
            self.emitted += 1

    def get(self, i):
        self.ensure(i)
        return self.slots[i % len(self.slots)], self.loaded[i]

    def release(self, i, ev):
        self.free[i % len(self.slots)] = ev


class Builder:
    def __init__(self, n_layers=DEPTH, debug=False, stop_after=99):
        self.stop_after = stop_after
        self.n_layers = n_layers
        self.debug = debug
        nc = bass.Bass("TRN2", target_bir_lowering=False)
        self.nc = nc
        self.s = Sched(nc)
        dk = "ExternalOutput" if debug else "Internal"

        def din(name, shape, dt):
            return nc.dram_tensor(name, shape, dt, kind="ExternalInput").ap()

        def dscr(name, shape, dt):
            return nc.dram_tensor(name, shape, dt, kind=dk).ap()

        self.x_in = din("xT", [NC_, 128, T], F32)
        WSH = {"w_in": [D, INW], "w_proj_a": [2048, D], "w_proj_b": [2048, D], "w_out": [D, D], "w_up": [D, DFF], "w_down": [DFF, D]}
        NEED = {"w_in": 1, "w_proj_a": 3, "w_proj_b": 3, "w_out": 3, "w_up": 4, "w_down": 4}
        self.W = {}
        self.wshape = {}
        for l in range(DEPTH):
            for nm, shp in WSH.items():
                needed = (l < n_layers - 1) or (l == n_layers - 1 and stop_after >= NEED[nm])
                shape = shp if needed else [8, 8]
                self.wshape[(nm, l)] = shape
                self.W[(nm, l)] = din(f"{nm}{l}", shape, F32)
        self.gains_d = din("gains", [128, DEPTH * 4 * NC_], F32)
        self.gsub_d = din("gsub", [128, DEPTH * 2], F32)
        self.lamv_d = din("lamv", [128, DEPTH * 4], F32)
        self.dtab_d = din("dtab", [128, (NT0 + NT1) * 512], F32)
        self.pm_d = din("pmask", [128, 64], F32)
        self.own_d = din("ownind", [128, 64], F32)
        self.ident_d = din("ident", [128, 128], BF16)
        self.ones_d = din("onesb", [128, 128], BF16)
        self.onesf_d = din("onesf", [128, 128], F32)
        self.esel_d = din("esel", [8, 8 * 128], BF16)
        self.out_d = nc.dram_tensor("outT", [NC_, 128, T], F32, kind="ExternalOutput").ap()

        self.xa_d = dscr("xa", [NC_, 128, T], F32)
        self.xb_d = dscr("xb", [NC_, 128, T], F32)
        self.q_d = dscr("qT", [NC_, 128, T], BF16)
        self.kv_loc = nc.dram_tensor("kv_loc", [8192, T], BF16).ap()
        self.kv_all = nc.dram_tensor("kv_all", [16384, T], BF16).ap()
        self.sg_d = dscr("sg", [64, 128, T], BF16)
        self.o_d = dscr("oT", [NC_, 128, T], BF16)
        self.mix_d = dscr("mix", [NC_, 128, T], F32)
        if debug:
            self.kvdbg_d = nc.dram_tensor("kvdbg", [16384, T], BF16, kind="ExternalOutput").ap()

    def sbuf(self, name, shape, dt):
        self._uid = getattr(self, "_uid", 0) + 1
        return self.nc.sbuf_tensor(f"{name}_u{self._uid}", shape, dt)

    def emit_casts(self):
        s = self.s
        self.cast_ev = {}
        first = [("winA", 0), ("winB", 0)]
        order = first + [(nm, l) for l in range(self.n_layers) for nm in PIECES if (nm, l) not in first]
        for (nm, l) in order:
            key = "cast0" if (nm, l) in first else "cast1"
            src_ = self.wsh[(nm, l)].rearrange("r (a b) -> r a b", b=2048)
            dst_ = self.wsb[(nm, l)].rearrange("r (a b) -> r a b", b=2048)
            self.cast_ev[(nm, l)] = s.dma("gpsimd", (lambda e, src_=src_, dst_=dst_: e.dma_start(out=dst_, in_=src_)), key)
        tot = s.sems["D_cast1"][1] if "D_cast1" in s.sems else 0
        for k in self.cast_ev:
            if k not in first:
                self.cast_ev[k] = Ev("D_cast1", tot)
        tot0 = s.sems["D_cast0"][1]
        for k in first:
            self.cast_ev[k] = Ev("D_cast0", tot0)

    def emit_gathers(self, keys):
        s = self.s
        for (nm, l) in keys:
            if l >= self.n_layers:
                continue
            a, b = self.wsb[(nm, l)], self.wfull[(nm, l)]
            self.wev[(nm, l)] = s.dma("gpsimd", (lambda e, a=a, b=b: e.collective_compute("AllGather", ALU.bypass, replica_groups=[list(range(8))], ins=[a], outs=[b])),
                                      f"ccw_{nm}{l}", waits=[self.cast_ev[(nm, l)]], inc=1)

    def build(self):
        nc, s = self.nc, self.s
        with contextlib.ExitStack() as gs:
            def sb(name, shape, dt):
                return gs.enter_context(self.sbuf(name, shape, dt))

            self.gains = sb("gains_s", [128, DEPTH * 4 * NC_], F32)
            self.gsub = sb("gsub_s", [128, DEPTH * 2], F32)
            self.lamv = sb("lamv_s", [128, DEPTH * 4], F32)
            self.ident = sb("ident_s", [128, 128], BF16)
            self.ones = sb("ones_s", [128, 128], BF16)
            self.onesf = sb("onesf_s", [128, 128], F32)
            self.esel = sb("esel_s", [8, 8 * 128], BF16)
            self.pm = sb("pm_s", [128, 64], F32)
            self.own = sb("own_s", [128, 64], F32)
            self.epsD = sb("epsD", [128, 1], F32)
            self.nlam = sb("nlam", [128, DEPTH], F32)
            self.gsl = sb("gsl", [128, DEPTH * 2], F32)
            self.lamt = sb("lamt", [128, 4], F32)
            self.banks = [gs.enter_context(nc.psum_tensor(f"bank{i}", [128, 512], F32)) for i in range(7)]
            self.bank7b = gs.enter_context(nc.psum_tensor("bank7", [128, 1024], BF16))

            cl = []
            for dst, src, k in [(self.gains, self.gains_d, "c0"), (self.gsub, self.gsub_d, "c1"),
                                (self.lamv, self.lamv_d, "c2"), (self.ident, self.ident_d, "c3"),
                                (self.ones, self.ones_d, "c4"), (self.onesf, self.onesf_d, "c5"),
                                (self.esel, self.esel_d, "c6"), (self.pm, self.pm_d, "c7"),
                                (self.own, self.own_d, "c8")]:
                cl.append(s.dma("sync", (lambda e, dst=dst, src=src: e.dma_start(out=dst[:], in_=src)), "c"))
            ev = s.op("vector", lambda e: e.memset(self.epsD[:], EPS))
            for l in range(self.n_layers):
                lam_init = 0.8 - 0.6 * math.exp(-0.3 * l)
                e1 = s.op("vector", lambda e, l=l: e.tensor_tensor(out=self.lamt[:, 0:1], in0=self.lamv[:, 4 * l:4 * l + 1], in1=self.lamv[:, 4 * l + 1:4 * l + 2], op=ALU.mult), waits=cl)
                e2 = s.op("vector", lambda e, l=l: e.tensor_tensor(out=self.lamt[:, 1:2], in0=self.lamv[:, 4 * l + 2:4 * l + 3], in1=self.lamv[:, 4 * l + 3:4 * l + 4], op=ALU.mult), waits=cl + [e1])
                e3 = s.op("tensor", lambda e: e.matmul(self.banks[0][:, 0:2], lhsT=self.onesf[:], rhs=self.lamt[:, 0:2], start=True, stop=True), waits=[e1, e2] + cl)
                e4 = s.op("scalar", lambda e: e.activation(out=self.lamt[:, 2:4], in_=self.banks[0][:, 0:2], func=AF.Exp), waits=[e3])
                e5 = s.op("vector", lambda e: e.tensor_tensor(out=self.lamt[:, 0:1], in0=self.lamt[:, 3:4], in1=self.lamt[:, 2:3], op=ALU.subtract), waits=[e4, e2])
                e6 = s.op("vector", lambda e, l=l, lam_init=lam_init: e.tensor_scalar(out=self.nlam[:, l:l + 1], in0=self.lamt[:, 0:1], scalar1=-lam_init, scalar2=None, op0=ALU.add), waits=[e5])
                e7 = s.op("vector", lambda e, l=l, lam_init=lam_init: e.tensor_scalar(out=self.gsl[:, 2 * l:2 * l + 2], in0=self.gsub[:, 2 * l:2 * l + 2], scalar1=(1.0 - lam_init), scalar2=None, op0=ALU.mult), waits=[e6])
                s.wait_only("tensor", [e5])
            s.barrier()

            x_cur = self.x_in
            for l in range(self.n_layers):
                last = (l == self.n_layers - 1)
                self.phase1(l, x_cur)
                s.barrier()
                if self.stop_after <= 1:
                    break
                self.phase2(l)
                s.barrier()
                if self.stop_after <= 2:
                    break
                self.phase3(l, x_cur)
                s.barrier()
                if self.stop_after <= 3:
                    break
                x_next = self.out_d if last else self.xb_d
                self.phase4(l, x_next)
                s.barrier()
                x_cur = x_next
            s.emit()
            s.close()
        return nc

    def prenorm(self, es, x_d, l, gidx, hT, tok0, ntok, ss_banks, tag):
        nc, s = self.nc, self.s
        ntg = ntok // 512
        NXS = 3
        xs = [es.enter_context(self.sbuf(f"xs{tag}{i}", [128, ntok], F32)) for i in range(NXS)]
        sqb = [es.enter_context(self.sbuf(f"sq{tag}{i}", [128, ntok], BF16)) for i in range(2)]
        rstd = es.enter_context(self.sbuf(f"rstd{tag}", [128, ntok], F32))
        xs_free = [None] * NXS
        sq_free = [None] * 2
        last_mm = None
        k = 0
        for c in range(NC_):
            sl = k % NXS
            ld = s.dma("sync", (lambda e, sl=sl, c=c: e.dma_start(out=xs[sl][:], in_=x_d[c, :, tok0:tok0 + ntok])), f"xs{sl}", waits=[xs_free[sl]])
            sq = s.op("scalar", (lambda e, sl=sl, c=c: e.activation(out=sqb[c % 2][:], in_=xs[sl][:], func=AF.Square)), waits=[ld, sq_free[c % 2]])
            for tg in range(ntg):
                last_mm = s.op("tensor", (lambda e, c=c, tg=tg: e.matmul(ss_banks[tg][:], lhsT=self.ones[:], rhs=sqb[c % 2][:, tg * 512:(tg + 1) * 512], start=(c == 0), stop=(c == NC_ - 1))), waits=[sq], sig=(tg == ntg - 1))
            xs_free[sl] = sq
            sq_free[c % 2] = last_mm
            k += 1
        ev = None
        for tg in range(ntg):
            e1 = s.op("scalar", (lambda e, tg=tg: e.activation(out=rstd[:, tg * 512:(tg + 1) * 512], in_=ss_banks[tg][:], func=AF.Ln, scale=1.0 / D, bias=self.epsD[:])), waits=[last_mm, ev])
            ev = s.op("scalar", (lambda e, tg=tg: e.activation(out=rstd[:, tg * 512:(tg + 1) * 512], in_=rstd[:, tg * 512:(tg + 1) * 512], func=AF.Exp, scale=-0.5)), waits=[e1])
        rs_ev = ev
        hev = []
        for c in range(NC_):
            sl = k % NXS
            ld = s.dma("sync", (lambda e, sl=sl, c=c: e.dma_start(out=xs[sl][:], in_=x_d[c, :, tok0:tok0 + ntok])), f"xs{sl}", waits=[xs_free[sl]])
            gcol = (l * 4 + gidx) * NC_ + c
            h = s.op("vector", (lambda e, sl=sl, c=c, gcol=gcol: e.scalar_tensor_tensor(out=hT[:, c, :], in0=xs[sl][:], scalar=self.gains[:, gcol:gcol + 1], in1=rstd[:], op0=ALU.mult, op1=ALU.mult)), waits=[ld, rs_ev])
            xs_free[sl] = h
            hev.append(h)
            k += 1
        return hev, last_mm

    def phase1(self, l, x_d):
        nc, s = self.nc, self.s
        B = self.banks
        with contextlib.ExitStack() as es:
            hT = es.enter_context(self.sbuf("hT1", [128, NC_, T], BF16))
            NS = 3
            wslots = [es.enter_context(self.sbuf(f"w1_{i}", [128, NC_, 512], BF16)) for i in range(NS)]
            NOB = 4
            ob = [es.enter_context(self.sbuf(f"ob1_{i}", [128, 512], BF16)) for i in range(NOB)]
            with contextlib.ExitStack() as es2:
                hev, _ = self.prenorm(es2, x_d, l, 0, hT, 0, T, [B[6], B[5]], "p1")
            ob_free = [None] * NOB
            order = list(range(40))
            NATCG = list(range(4, 8)) + list(range(16, 20)) + list(range(8, 12)) + list(range(20, 24)) + \
                list(range(0, 4)) + list(range(12, 16)) + list(range(24, 40))
            wv = self.W[("w_in", l)].rearrange("(kc p) c -> p kc c", p=128)

            def mk_task(cg):
                c0 = NATCG[cg] * 512

                def t(slot):
                    return [(lambda e, q=q: e.dma_start(out=slot[:, q * 8:(q + 1) * 8, :], in_=wv[:, q * 8:(q + 1) * 8, c0:c0 + 512])) for q in range(4)]
                return t
            ws = WStream(s, "w1s", wslots, [mk_task(cg) for cg in order], lookahead=2, eng="gpsimd")
            kvT = self.kv_loc[0:4096, :].rearrange("(j p) t -> j p t", p=128)
            vloc = self.kv_loc[4096:8192, :].rearrange("(t a) b -> t (a b)", a=4)
            pbank = [[B[0], B[1]], [B[2], B[3]]]
            pfree = [[None, None], [None, None]]
            kv_stores = []
            nout = 0
            cc_ev = None
            for i, cg in enumerate(order):
                wt, wev = ws.get(i)
                last_mm = None
                if 8 <= cg < 16:
                    vcol0 = (cg - 8) * 512
                    for tt in range(8):
                        pb = nout % 2
                        bank = pbank[pb][0]
                        for kc in range(NC_):
                            last_mm = s.op("tensor", (lambda e, bank=bank, kc=kc, tt=tt, wt=wt: e.matmul(bank[:], lhsT=hT[:, kc, tt * 128:(tt + 1) * 128], rhs=wt[:, kc, :], start=(kc == 0), stop=(kc == NC_ - 1))),
                                           waits=([wev, pfree[pb][0]] + hev) if kc == 0 else (), sig=(kc == NC_ - 1))
                        o = nout % NOB
                        eng = "vector" if nout % 2 == 0 else "scalar"
                        if eng == "vector":
                            ev = s.op("vector", (lambda e, o=o, bank=bank: e.tensor_copy(out=ob[o][:], in_=bank[:])), waits=[last_mm, ob_free[o]])
                        else:
                            ev = s.op("scalar", (lambda e, o=o, bank=bank: e.copy(out=ob[o][:], in_=bank[:])), waits=[last_mm, ob_free[o]])
                        pfree[pb][0] = ev
                        st = s.dma("sync", (lambda e, o=o, tt=tt, vcol0=vcol0: e.dma_start(out=vloc[tt * 128:(tt + 1) * 128, vcol0:vcol0 + 512], in_=ob[o][:])), f"ob1_{o}", waits=[ev])
                        ob_free[o] = st
                        kv_stores.append(st)
                        nout += 1
                else:
                    for ec in range(4):
                        ch = cg * 4 + ec
                        pb = nout % 2
                        for kc in range(NC_):
                            for tg in range(2):
                                bank = pbank[pb][tg]
                                last_mm = s.op("tensor", (lambda e, bank=bank, kc=kc, tg=tg, ec=ec, wt=wt: e.matmul(bank[:], lhsT=wt[:, kc, ec * 128:(ec + 1) * 128], rhs=hT[:, kc, tg * 512:(tg + 1) * 512], start=(kc == 0), stop=(kc == NC_ - 1))),
                                               waits=([wev, pfree[pb][tg]] + hev) if kc == 0 else (), sig=(kc == NC_ - 1 and tg == 1))
                        if ch < 32:
                            dst, sig_ = kvT[ch], False
                        elif ch < 96:
                            dst, sig_ = self.q_d[ch - 64], False
                        else:
                            dst, sig_ = self.sg_d[ch - 96], True
                        is_kv = ch < 32
                        for tg in range(2):
                            bank = pbank[pb][tg]
                            o = (nout * 2 + tg) % NOB
                            if sig_:
                                ev = s.op("scalar", (lambda e, o=o, bank=bank: e.activation(out=ob[o][:], in_=bank[:], func=AF.Sigmoid)), waits=[last_mm, ob_free[o]])
                            elif tg == 0:
                                ev = s.op("vector", (lambda e, o=o, bank=bank: e.tensor_copy(out=ob[o][:], in_=bank[:])), waits=[last_mm, ob_free[o]])
                            else:
                                ev = s.op("scalar", (lambda e, o=o, bank=bank: e.copy(out=ob[o][:], in_=bank[:])), waits=[last_mm, ob_free[o]])
                            pfree[pb][tg] = ev
                            st = s.dma("sync", (lambda e, o=o, dst=dst, tg=tg: e.dma_start(out=dst[:, tg * 512:(tg + 1) * 512], in_=ob[o][:])), f"ob1_{o}", waits=[ev])
                            ob_free[o] = st
                            if is_kv:
                                kv_stores.append(st)
                        nout += 1
                ws.release(i, last_mm)
                if i == 15:
                    for q in range(8):
                        cc_ev = s.dma("gpsimd", (lambda e, q=q: e.collective_compute("AllGather", ALU.bypass, replica_groups=[[0, 1], [2, 3], [4, 5], [6, 7]],
                                                                                    ins=[self.kv_loc[q * 1024:(q + 1) * 1024, :]], outs=[self.kv_all[q * 2048:(q + 1) * 2048, :]])), "cc", waits=kv_stores, inc=1)
            self.cc_ev = cc_ev
            if self.debug:
                for q in range(16):
                    s.dma("sync", (lambda e, q=q: e.dma_start(out=self.kvdbg_d[q * 1024:(q + 1) * 1024, :], in_=self.kv_all[q * 1024:(q + 1) * 1024, :])), "kvdbg", waits=[cc_ev])

    def phase2(self, l):
        nc, s = self.nc, self.s
        B = self.banks
        with contextlib.ExitStack() as es:
            dtab = es.enter_context(self.sbuf("dtab_s", [128, NT0 + NT1, 512], F32))
            ld_d = s.dma("sync", (lambda e: e.dma_start(out=dtab[:].rearrange("p a b -> p (a b)"), in_=self.dtab_d)), "dtab")
            KT = [es.enter_context(self.sbuf(f"KT{i}", [128, 2, T], BF16)) for i in range(2)]
            QT = [es.enter_context(self.sbuf(f"QT{i}", [128, T], BF16)) for i in range(2)]
            VT = [es.enter_context(self.sbuf(f"VT{i}", [128, 16, 256], BF16)) for i in range(2)]
            NST = 3
            st_ = [es.enter_context(self.sbuf(f"st{i}", [128, 512], F32)) for i in range(NST)]
            pt_ = [es.enter_context(self.sbuf(f"pt{i}", [128, 512], BF16)) for i in range(NST)]
            st_free = [None] * NST
            pt_free = [None] * NST
            rs_ = [es.enter_context(self.sbuf(f"rs{i}", [128, 512], F32)) for i in range(2)]
            oo_ = [es.enter_context(self.sbuf(f"oo{i}", [128, 512], BF16)) for i in range(4)]
            oo_free = [None] * 4
            km = es.enter_context(self.sbuf("km", [128, 8], F32))
            kmh = es.enter_context(self.sbuf("kmh", [128, 8], BF16))
            kmhf = es.enter_context(self.sbuf("kmhf", [128, 8], F32))
            kml = es.enter_context(self.sbuf("kml", [128, 8], BF16))
            gm = es.enter_context(self.sbuf("gm", [128, 64], F32))
            top8 = es.enter_context(self.sbuf("top8", [128, 64], F32))
            selb = es.enter_context(self.sbuf("selb", [128, 64], BF16))
            self_f = es.enter_context(self.sbuf("self_f", [128, 64], F32))
            selT = [es.enter_context(self.sbuf(f"selT{i}", [8, T], BF16)) for i in range(2)]
            A_ = [es.enter_context(self.sbuf(f"A{i}", [128, 2, 512], F32)) for i in range(2)]
            od = es.enter_context(self.sbuf("od", [128, 2, 512], F32))
            osq = es.enter_context(self.sbuf("osq", [128, 2, 512], BF16))
            rsub = es.enter_context(self.sbuf("rsub", [128, 512], F32))
            eps_s = es.enter_context(self.sbuf("eps_s", [128, 1], F32))
            s.op("vector", lambda e: e.memset(eps_s[:], EPS))
            bank7f = self.bank7b[:].bitcast(F32)

            cc = self.cc_ev
            ld_free = [None, None]
            nst = 0
            noo = 0

            def load_head(par, kchunk, qchunk, vcol0, vw):
                w = [ld_free[par], cc]
                evs = []
                kq, kr = kchunk // 8, (kchunk % 8) * 128
                for r in range(2):
                    r0 = kq * 2048 + r * 1024 + kr
                    evs.append(s.dma("sync", (lambda e, r=r, r0=r0: e.dma_start(out=KT[par][:, r, :], in_=self.kv_all[r0:r0 + 128, :])), f"kt{par}", waits=w))
                evs.append(s.dma("sync", (lambda e: e.dma_start(out=QT[par][:], in_=self.q_d[qchunk])), f"qt{par}", waits=w))
                for r in range(2):
                    for q4 in range(4):
                        r0 = (4 + q4) * 2048 + r * 1024
                        vsrc = self.kv_all[r0:r0 + 1024, :].rearrange("(t a) b -> t (a b)", a=4)
                        k0 = r * 8 + q4 * 2
                        evs.append(s.dma("sync", (lambda e, vsrc=vsrc, k0=k0: e.dma_start(out=VT[par][:, k0:k0 + 2, 0:vw], in_=vsrc[:, vcol0:vcol0 + vw].rearrange("(k p) d -> p k d", p=128))), f"vt{par}", waits=w))
                return evs

            def score_tiles(par, qt, c_h, s_banks, sel_par, on_pt):
                nonlocal nst
                nkt = NT0 if qt == 0 else NT1
                base = 0 if qt == 0 else NT0
                for kt in range(nkt):
                    sbk = s_banks[kt % 2]
                    r, kk = kt // 8, kt % 8
                    mm = s.op("tensor", (lambda e, sbk=sbk, r=r, kk=kk: e.matmul(sbk[:], lhsT=KT[par][:, r, kk * 128:(kk + 1) * 128], rhs=QT[par][:, qt * 512:(qt + 1) * 512], start=True, stop=(sel_par is None))),
                              waits=self._sfree[kt % 2:kt % 2 + 1] + self._ldev, sig=(sel_par is None))
                    if sel_par is not None:
                        n = kt // 2
                        mm = s.op("tensor", (lambda e, sbk=sbk, n=n: e.matmul(sbk[:], lhsT=self.esel[:, n * 128:(n + 1) * 128], rhs=selT[sel_par][:, qt * 512:(qt + 1) * 512], start=False, stop=True)), waits=self._selev)
                    i = nst % NST
                    nst += 1
                    ea = s.op("vector", (lambda e, i=i, sbk=sbk, kt=kt: e.scalar_tensor_tensor(out=st_[i][:], in0=dtab[:, base + kt, :], scalar=float(c_h), in1=sbk[:], op0=ALU.mult, op1=ALU.add)), waits=[mm, st_free[i], ld_d])
                    self._sfree[kt % 2] = ea
                    ee = s.op("scalar", (lambda e, i=i: e.activation(out=pt_[i][:], in_=st_[i][:], func=AF.Exp, scale=SCALE)), waits=[ea, pt_free[i]])
                    st_free[i] = ee
                    evc = on_pt(kt, pt_[i], ee, kt == 0, kt == nkt - 1)
                    pt_free[i] = evc

            self._sfree = [None, None]
            heads = list(range(16))
            self._ldev = []
            nxt = load_head(0, 0, 0, 0 * 128, 128)
            acc_free = [[None, None], [None, None]]
            it = 0
            gate_free = None
            selT_free = [None, None]
            for hi, h in enumerate(heads):
                par = hi % 2
                ldev = nxt
                if hi + 1 < len(heads):
                    h2 = heads[hi + 1]
                    nxt = load_head(1 - par, h2, h2, h2 * 128, 128)
                slope = 2.0 ** (-8.0 * (h + 1) / 16.0)
                c_h = slope / SCALE
                e1 = s.op("vector", (lambda e, par=par: e.tensor_reduce(out=km[:], in_=KT[par][:].rearrange("p r (n k) -> p (r n) k", k=256), axis=AX.X, op=ALU.add)), waits=ldev + [gate_free])
                e2 = s.op("vector", lambda e: e.tensor_scalar(out=km[:], in0=km[:], scalar1=1.0 / 256.0, scalar2=None, op0=ALU.mult), waits=[e1])
                e3 = s.op("vector", lambda e: e.tensor_copy(out=kmh[:], in_=km[:]), waits=[e2])
                e4 = s.op("vector", lambda e: e.tensor_copy(out=kmhf[:], in_=kmh[:]), waits=[e3])
                e5 = s.op("vector", lambda e: e.tensor_tensor(out=kml[:], in0=km[:], in1=kmhf[:], op=ALU.subtract), waits=[e4])
                gmm = None
                for q8 in range(8):
                    s.op("tensor", (lambda e, q8=q8, par=par: e.matmul(B[6][:, q8 * 8:(q8 + 1) * 8], lhsT=QT[par][:, q8 * 128:(q8 + 1) * 128], rhs=kmh[:], start=True, stop=False)), waits=[e5, gate_free] + ldev, sig=False)
                    gmm = s.op("tensor", (lambda e, q8=q8, par=par: e.matmul(B[6][:, q8 * 8:(q8 + 1) * 8], lhsT=QT[par][:, q8 * 128:(q8 + 1) * 128], rhs=kml[:], start=False, stop=True)), sig=(q8 == 7))
                e6 = s.op("vector", lambda e: e.tensor_tensor(out=gm[:], in0=B[6][:, 0:64], in1=self.pm[:], op=ALU.add), waits=[gmm])
                gate_free = e6
                ev = e6
                for q8 in range(8):
                    ev = s.op("vector", (lambda e, q8=q8: e.max(out=top8[:, q8 * 8:(q8 + 1) * 8], in_=gm[:, q8 * 8:(q8 + 1) * 8])), waits=[ev])
                for q8 in range(8):
                    ev = s.op("vector", (lambda e, q8=q8: e.tensor_scalar(out=self_f[:, q8 * 8:(q8 + 1) * 8], in0=gm[:, q8 * 8:(q8 + 1) * 8], scalar1=top8[:, q8 * 8 + 2:q8 * 8 + 3], scalar2=None, op0=ALU.is_ge)), waits=[ev])
                ev = s.op("vector", lambda e: e.tensor_tensor(out=self_f[:], in0=self_f[:], in1=self.own[:], op=ALU.max), waits=[ev])
                ev = s.op("vector", lambda e: e.tensor_scalar(out=selb[:], in0=self_f[:], scalar1=-1.0, scalar2=SELBIG, op0=ALU.add, op1=ALU.mult), waits=[ev, self._b7free])
                tr = None
                for q8 in range(8):
                    tr = s.op("tensor", (lambda e, q8=q8: e.transpose(out=self.bank7b[0:8, q8 * 128:(q8 + 1) * 128], in_=selb[:, q8 * 8:(q8 + 1) * 8], identity=self.ident[:])), waits=[ev, self._b7free] if q8 == 0 else (), sig=(q8 == 7))
                evs = s.op("vector", (lambda e, par=par: e.tensor_copy(out=selT[par][:], in_=self.bank7b[0:8, :])), waits=[tr, selT_free[par]])
                self._b7free = evs
                self._selev = [evs]
                self._ldev = ldev
                last_use = None
                for qt in range(2):
                    ps = it % 2
                    it += 1
                    Ob, Sb = B[2 + ps], B[4 + ps]

                    def on_pt(kt, pt, ee, first, lastk, Ob=Ob, Sb=Sb, par=par, ps=ps):
                        s.op("tensor", (lambda e: e.matmul(Ob[:], lhsT=VT[par][:, kt, 0:128], rhs=pt[:], start=first, stop=lastk)), waits=[ee] + ([acc_free[ps][0]] if first else []), sig=False)
                        return s.op("tensor", (lambda e: e.matmul(Sb[:], lhsT=self.ones[:], rhs=pt[:], start=first, stop=lastk)), waits=[acc_free[ps][1]] if first else ())
                    self._last_pv = None

                    def on_pt2(kt, pt, ee, first, lastk):
                        ev = on_pt(kt, pt, ee, first, lastk)
                        self._last_pv = ev
                        return ev
                    score_tiles(par, qt, c_h, [B[0], B[1]], par, on_pt2)
                    lp = self._last_pv
                    r = it % 2
                    e1 = s.op("vector", (lambda e, r=r, Sb=Sb: e.reciprocal(out=rs_[r][:], in_=Sb[:])), waits=[lp])
                    o = noo % 4
                    noo += 1
                    e2 = s.op("vector", (lambda e, r=r, o=o, Ob=Ob: e.tensor_tensor(out=oo_[o][:], in0=Ob[:], in1=rs_[r][:], op=ALU.mult)), waits=[e1, oo_free[o]])
                    acc_free[ps] = [e2, e1]
                    st = s.dma("sync", (lambda e, o=o, h=h, qt=qt: e.dma_start(out=self.o_d[h][:, qt * 512:(qt + 1) * 512], in_=oo_[o][:])), f"oo{o}", waits=[e2])
                    oo_free[o] = st
                    last_use = lp
                selT_free[par] = last_use
                ld_free[par] = last_use
            s.barrier()
            ld_free = [None, None]
            self._selev = []
            nld = 0
            seq = [(h, m) for h in range(8) for m in range(2)]
            nxt = load_head(0, 16 + 0, 16 + 0, 2048, 256)
            it = 0
            acc_free = [[None, None, None], [None, None, None]]
            A_free = [None, None]
            od_free = None
            for si, (h, m) in enumerate(seq):
                par = si % 2
                ldev = nxt
                if si + 1 < len(seq):
                    h2, m2 = seq[si + 1]
                    nxt = load_head(1 - par, 16 + 2 * h2 + m2, 16 + 2 * h2 + m2, 2048 + h2 * 256, 256)
                slope = 2.0 ** (-8.0 * (h + 1) / 8.0)
                c_h = slope / SCALE
                self._ldev = ldev
                last_use = None
                for qt in range(2):
                    ps = it % 2
                    it += 1
                    O0, O1, Sb = (B[2], B[3], B[4]) if ps == 0 else (B[5], B[6], bank7f)

                    def on_pt(kt, pt, ee, first, lastk, O0=O0, O1=O1, Sb=Sb, par=par, ps=ps):
                        s.op("tensor", (lambda e: e.matmul(O0[:], lhsT=VT[par][:, kt, 0:128], rhs=pt[:], start=first, stop=lastk)), waits=[ee] + ([acc_free[ps][0]] if first else []), sig=False)
                        s.op("tensor", (lambda e: e.matmul(O1[:], lhsT=VT[par][:, kt, 128:256], rhs=pt[:], start=first, stop=lastk)), waits=[acc_free[ps][1]] if first else (), sig=False)
                        ev = s.op("tensor", (lambda e: e.matmul(Sb[:], lhsT=self.ones[:], rhs=pt[:], start=first, stop=lastk)), waits=[acc_free[ps][2]] if first else ())
                        self._last_pv = ev
                        return ev
                    score_tiles(par, qt, c_h, [B[0], B[1]], None, on_pt)
                    lp = self._last_pv
                    r = it % 2
                    e1 = s.op("vector", (lambda e, r=r, Sb=Sb: e.reciprocal(out=rs_[r][:], in_=Sb[:])), waits=[lp])
                    a = A_[qt]
                    if m == 0:
                        e2 = s.op("vector", (lambda e, r=r, a=a, O0=O0: e.tensor_tensor(out=a[:, 0, :], in0=O0[:], in1=rs_[r][:], op=ALU.mult)), waits=[e1, A_free[qt]])
                        e3 = s.op("vector", (lambda e, r=r, a=a, O1=O1: e.tensor_tensor(out=a[:, 1, :], in0=O1[:], in1=rs_[r][:], op=ALU.mult)), waits=[e1, A_free[qt]])
                        acc_free[ps] = [e2, e3, e1]
                    else:
                        e2 = s.op("vector", (lambda e, r=r, O0=O0: e.tensor_tensor(out=od[:, 0, :], in0=O0[:], in1=rs_[r][:], op=ALU.mult)), waits=[e1, od_free])
                        e3 = s.op("vector", (lambda e, r=r, O1=O1: e.tensor_tensor(out=od[:, 1, :], in0=O1[:], in1=rs_[r][:], op=ALU.mult)), waits=[e1, od_free])
                        acc_free[ps] = [e2, e3, e1]
                        e4 = s.op("vector", (lambda e, a=a: e.scalar_tensor_tensor(out=od[:].rearrange("p a b -> p (a b)"), in0=od[:].rearrange("p a b -> p (a b)"), scalar=self.nlam[:, l:l + 1], in1=a[:].rearrange("p a b -> p (a b)"), op0=ALU.mult, op1=ALU.add)), waits=[e2, e3])
                        A_free[qt] = e4
                        e5 = s.op("scalar", (lambda e: e.activation(out=osq[:].rearrange("p a b -> p (a b)"), in_=od[:].rearrange("p a b -> p (a b)"), func=AF.Square)), waits=[e4, self._osq_free])
                        s.op("tensor", (lambda e, Sb=Sb: e.matmul(Sb[:], lhsT=self.ones[:], rhs=osq[:, 0, :], start=True, stop=False)), waits=[e5, e1], sig=False)
                        e6 = s.op("tensor", (lambda e, Sb=Sb: e.matmul(Sb[:], lhsT=self.ones[:], rhs=osq[:, 1, :], start=False, stop=True)))
                        self._osq_free = e6
                        e7 = s.op("scalar", (lambda e, Sb=Sb: e.activation(out=rsub[:], in_=Sb[:], func=AF.Ln, scale=1.0 / 256.0, bias=eps_s[:])), waits=[e6, self._rsub_free])
                        e8 = s.op("scalar", (lambda e: e.activation(out=rsub[:], in_=rsub[:], func=AF.Exp, scale=-0.5)), waits=[e7])
                        acc_free[ps][2] = e7
                        evo = None
                        for c in range(2):
                            o = noo % 4
                            noo += 1
                            e9 = s.op("vector", (lambda e, c=c, o=o: e.scalar_tensor_tensor(out=oo_[o][:], in0=od[:, c, :], scalar=self.gsl[:, 2 * l + c:2 * l + c + 1], in1=rsub[:], op0=ALU.mult, op1=ALU.mult)), waits=[e8, e4, oo_free[o]])
                            st = s.dma("sync", (lambda e, o=o, h=h, c=c, qt=qt: e.dma_start(out=self.o_d[16 + 2 * h + c][:, qt * 512:(qt + 1) * 512], in_=oo_[o][:])), f"oo{o}", waits=[e9])
                            oo_free[o] = st
                            evo = e9
                        od_free = evo
                        self._rsub_free = evo
                    last_use = lp
                ld_free[par] = last_use

    _b7free = None
    _osq_free = None
    _rsub_free = None

    def phase3(self, l, x_d):
        nc, s = self.nc, self.s
        B = self.banks
        bank7f = self.bank7b[:].bitcast(F32)
        with contextlib.ExitStack() as es:
            yT = es.enter_context(self.sbuf("yT", [128, NC_, T], BF16))
            yev = []
            with contextlib.ExitStack() as es2:
                oT = es2.enter_context(self.sbuf("oT", [128, NC_, T], BF16))
                lo = []
                for c in range(NC_):
                    lo.append(s.dma("sync", (lambda e, c=c: e.dma_start(out=oT[:, c, :], in_=self.o_d[c])), f"oT{c % 4}"))
                wa_sl = [es2.enter_context(self.sbuf(f"wa{i}", [128, 16, 256], BF16)) for i in range(3)]
                wb_sl = [es2.enter_context(self.sbuf(f"wb{i}", [128, 16, 256], BF16)) for i in range(3)]
                wav = self.W[("w_proj_a", l)].rearrange("(kc p) c -> p kc c", p=128)
                wbv = self.W[("w_proj_b", l)].rearrange("(kc p) c -> p kc c", p=128)

                def mk(wv_, d2):
                    def t(slot):
                        return lambda e: e.dma_start(out=slot[:], in_=wv_[:, :, d2 * 256:(d2 + 1) * 256])
                    return t
                wsa = WStream(s, "wpa", wa_sl, [mk(wav, d2) for d2 in range(NC_ // 2)], lookahead=2, eng="gpsimd")
                wsb = WStream(s, "wpb", wb_sl, [mk(wbv, d2) for d2 in range(NC_ // 2)], lookahead=2, eng="gpsimd")
                sga = [es2.enter_context(self.sbuf(f"sga{i}", [128, T], BF16)) for i in range(2)]
                sgb = [es2.enter_context(self.sbuf(f"sgb{i}", [128, T], BF16)) for i in range(2)]
                t1 = [es2.enter_context(self.sbuf(f"t1_{i}", [128, 512], F32)) for i in range(2)]
                t2 = [es2.enter_context(self.sbuf(f"t2_{i}", [128, 512], F32)) for i in range(2)]
                sg_free = [None, None]
                t_free = [None, None]
                pfree = [[None] * 4, [None] * 4]
                nt = 0
                for dc in range(NC_):
                    pb = dc % 2
                    banks = [B[0], B[1], B[2], B[3]] if pb == 0 else [B[4], B[5], B[6], bank7f]
                    wa, eva = wsa.get(dc // 2)
                    wb, evb = wsb.get(dc // 2)
                    wc0 = (dc % 2) * 128
                    g = dc % 2
                    lga = s.dma("sync", (lambda e, g=g, dc=dc: e.dma_start(out=sga[g][:], in_=self.sg_d[dc])), f"sga{g}", waits=[sg_free[g]])
                    lgb = s.dma("sync", (lambda e, g=g, dc=dc: e.dma_start(out=sgb[g][:], in_=self.sg_d[32 + dc])), f"sgb{g}", waits=[sg_free[g]])
                    lma = lmb = None
                    for kc in range(16):
                        for tg in range(2):
                            lma = s.op("tensor", (lambda e, bk=banks[tg], kc=kc, tg=tg, wa=wa, wc0=wc0: e.matmul(bk[:], lhsT=wa[:, kc, wc0:wc0 + 128], rhs=oT[:, kc, tg * 512:(tg + 1) * 512], start=(kc == 0), stop=(kc == 15))),
                                       waits=([eva, pfree[pb][tg]] + lo) if kc == 0 else (), sig=(kc == 15 and tg == 1))
                    for kc in range(16):
                        for tg in range(2):
                            lmb = s.op("tensor", (lambda e, bk=banks[2 + tg], kc=kc, tg=tg, wb=wb, wc0=wc0: e.matmul(bk[:], lhsT=wb[:, kc, wc0:wc0 + 128], rhs=oT[:, 16 + kc, tg * 512:(tg + 1) * 512], start=(kc == 0), stop=(kc == 15))),
                                       waits=([evb, pfree[pb][2 + tg]]) if kc == 0 else (), sig=(kc == 15 and tg == 1))
                    if dc % 2 == 1:
                        wsa.release(dc // 2, lma)
                        wsb.release(dc // 2, lmb)
                    last = None
                    for tg in range(2):
                        ti = nt % 2
                        nt += 1
                        e1 = s.op("vector", (lambda e, ti=ti, bk=banks[tg], g=g, tg=tg: e.tensor_tensor(out=t1[ti][:], in0=bk[:], in1=sga[g][:, tg * 512:(tg + 1) * 512], op=ALU.mult)), waits=[lma, lga, t_free[ti]])
                        e2 = s.op("vector", (lambda e, ti=ti, bk=banks[2 + tg], g=g, tg=tg: e.tensor_tensor(out=t2[ti][:], in0=bk[:], in1=sgb[g][:, tg * 512:(tg + 1) * 512], op=ALU.mult)), waits=[lmb, lgb, t_free[ti]])
                        pfree[pb][tg] = e1
                        pfree[pb][2 + tg] = e2
                        e3 = s.op("vector", (lambda e, ti=ti, dc=dc, tg=tg: e.tensor_tensor(out=yT[:, dc, tg * 512:(tg + 1) * 512], in0=t1[ti][:], in1=t2[ti][:], op=ALU.add)), waits=[e1, e2])
                        t_free[ti] = e3
                        yev.append(e3)
                        last = e2
                    sg_free[g] = last
            s.barrier()
            with contextlib.ExitStack() as es3:
                wo_sl = [es3.enter_context(self.sbuf(f"wo{i}", [128, NC_, 256], BF16)) for i in range(3)]
                wov = self.W[("w_out", l)].rearrange("(kc p) c -> p kc c", p=128)

                def mko(d2):
                    def t(slot):
                        return [(lambda e, q=q: e.dma_start(out=slot[:, q * 16:(q + 1) * 16, :], in_=wov[:, q * 16:(q + 1) * 16, d2 * 256:(d2 + 1) * 256])) for q in range(2)]
                    return t
                wso = WStream(s, "wo", wo_sl, [mko(d2) for d2 in range(NC_ // 2)], lookahead=2, eng="gpsimd")
                stg = [es3.enter_context(self.sbuf(f"mst{i}", [128, 512], F32)) for i in range(4)]
                sq = [es3.enter_context(self.sbuf(f"msq{i}", [128, 512], BF16)) for i in range(4)]
                stg_free = [None] * 4
                self._stg_rd = [None] * 4
                sq_free = [None] * 4
                pfree = [[None, None], [None, None]]
                n = 0
                ss_last = None
                for dc in range(NC_):
                    pb = dc % 2
                    banks = [B[0], B[1]] if pb == 0 else [B[2], B[3]]
                    wo, evo = wso.get(dc // 2)
                    wc0 = (dc % 2) * 128
                    lm = None
                    for kc in range(NC_):
                        for tg in range(2):
                            lm = s.op("tensor", (lambda e, bk=banks[tg], kc=kc, tg=tg, wo=wo, wc0=wc0: e.matmul(bk[:], lhsT=wo[:, kc, wc0:wc0 + 128], rhs=yT[:, kc, tg * 512:(tg + 1) * 512], start=(kc == 0), stop=(kc == NC_ - 1))),
                                      waits=([evo, pfree[pb][tg]] + yev) if kc == 0 else (), sig=(kc == NC_ - 1 and tg == 1))
                    if dc % 2 == 1:
                        wso.release(dc // 2, lm)
                    for tg in range(2):
                        i = n % 4
                        n += 1
                        e1 = s.op("vector", (lambda e, i=i, bk=banks[tg]: e.tensor_copy(out=stg[i][:], in_=bk[:])), waits=[lm, stg_free[i], self._stg_rd[i]])
                        pfree[pb][tg] = e1
                        e2 = s.op("scalar", (lambda e, i=i: e.activation(out=sq[i][:], in_=stg[i][:], func=AF.Square)), waits=[e1, sq_free[i]])
                        st = s.dma("sync", (lambda e, i=i, dc=dc, tg=tg: e.dma_start(out=self.mix_d[dc][:, tg * 512:(tg + 1) * 512], in_=stg[i][:])), f"mst{i}", waits=[e1])
                        stg_free[i] = st
                        self._stg_rd[i] = e2
                        ssb = B[5] if tg == 0 else B[6]
                        ss_last = s.op("tensor", (lambda e, i=i, ssb=ssb, dc=dc: e.matmul(ssb[:], lhsT=self.ones[:], rhs=sq[i][:], start=(dc == 0), stop=(dc == NC_ - 1))), waits=[e2])
                        sq_free[i] = ss_last
            s.barrier()
            self.resid(l, 1, self.mix_d, x_d, self.xa_d, [B[5], B[6]], 0, T)

    def resid(self, l, gidx, m_d, x_d, out_d, ss_banks, tok0, ntok, m_sb=None):
        nc, s = self.nc, self.s
        ntg = ntok // 512
        with contextlib.ExitStack() as es:
            rstd = es.enter_context(self.sbuf("rstd_r", [128, ntok], F32))
            ev = None
            for tg in range(ntg):
                e1 = s.op("scalar", (lambda e, tg=tg: e.activation(out=rstd[:, tg * 512:(tg + 1) * 512], in_=ss_banks[tg][:], func=AF.Ln, scale=1.0 / D, bias=self.epsD[:])), waits=[ev])
                ev = s.op("scalar", (lambda e, tg=tg: e.activation(out=rstd[:, tg * 512:(tg + 1) * 512], in_=rstd[:, tg * 512:(tg + 1) * 512], func=AF.Exp, scale=-0.5)), waits=[e1])
            rs_ev = ev
            NB = 3
            mb = [es.enter_context(self.sbuf(f"rm{i}", [128, ntok], F32)) for i in range(NB)] if m_sb is None else None
            xb = [es.enter_context(self.sbuf(f"rx{i}", [128, ntok], F32)) for i in range(NB)]
            m_free = [None] * NB
            x_free = [None] * NB
            for c in range(NC_):
                i = c % NB
                waits = [rs_ev]
                if m_sb is None:
                    lm = s.dma("sync", (lambda e, i=i, c=c: e.dma_start(out=mb[i][:], in_=m_d[c][:, tok0:tok0 + ntok])), f"rm{i}", waits=[m_free[i]])
                    msrc = mb[i][:]
                    waits.append(lm)
                else:
                    msrc = m_sb[:, c, :]
                lx = s.dma("sync", (lambda e, i=i, c=c: e.dma_start(out=xb[i][:], in_=x_d[c][:, tok0:tok0 + ntok])), f"rx{i}", waits=[x_free[i]])
                gcol = (l * 4 + gidx) * NC_ + c
                if m_sb is None:
                    e1 = s.op("vector", (lambda e, i=i, gcol=gcol, msrc=msrc: e.scalar_tensor_tensor(out=mb[i][:], in0=msrc, scalar=self.gains[:, gcol:gcol + 1], in1=rstd[:], op0=ALU.mult, op1=ALU.mult)), waits=waits)
                    e2 = s.op("vector", (lambda e, i=i: e.tensor_tensor(out=xb[i][:], in0=xb[i][:], in1=mb[i][:], op=ALU.add)), waits=[e1, lx])
                    m_free[i] = e2
                else:
                    e1 = s.op("vector", (lambda e, c=c, gcol=gcol, msrc=msrc: e.scalar_tensor_tensor(out=msrc, in0=msrc, scalar=self.gains[:, gcol:gcol + 1], in1=rstd[:], op0=ALU.mult, op1=ALU.mult)), waits=waits)
                    e2 = s.op("vector", (lambda e, i=i, msrc=msrc: e.tensor_tensor(out=xb[i][:], in0=xb[i][:], in1=msrc, op=ALU.add)), waits=[e1, lx])
                st = s.dma("sync", (lambda e, i=i, c=c: e.dma_start(out=out_d[c][:, tok0:tok0 + ntok], in_=xb[i][:])), f"rxs{i}", waits=[e2])
                x_free[i] = st

    def phase4(self, l, out_d):
        nc, s = self.nc, self.s
        B = self.banks
        bank7f = self.bank7b[:].bitcast(F32)
        HT = 512
        NFB = DFF // 512
        NF = DFF // 128
        for hh in range(2):
            tok0 = hh * HT
            with contextlib.ExitStack() as es:
                hT = es.enter_context(self.sbuf("hT4", [128, NC_, HT], BF16))
                macc = es.enter_context(self.sbuf("macc", [128, NC_, HT], F32))
                wu_sl = [es.enter_context(self.sbuf(f"wu{i}", [128, 16, 256], BF16)) for i in range(4)]
                wd_sl = [es.enter_context(self.sbuf(f"wd{i}", [128, D], BF16)) for i in range(5)]
                uT = [es.enter_context(self.sbuf(f"uT{i}", [128, 4, HT], BF16)) for i in range(2)]
                rb = [es.enter_context(self.sbuf(f"rb{i}", [128, HT], F32)) for i in range(2)]
                sqm = [es.enter_context(self.sbuf(f"sqm{i}", [128, HT], BF16)) for i in range(2)]
                with contextlib.ExitStack() as es2:
                    hev, _ = self.prenorm(es2, self.xa_d, l, 2, hT, tok0, HT, [B[6]], "p4")
                wuv = self.W[("w_up", l)].rearrange("(kh kc p) c -> kh p kc c", p=128, kh=2)
                wdv = self.W[("w_down", l)].rearrange("(fc p) (a b) -> fc p a b", p=128, b=2048)

                def mku(P, kh):
                    def t(slot):
                        return lambda e: e.dma_start(out=slot[:], in_=wuv[kh][:, :, P * 256:(P + 1) * 256])
                    return t

                def mkd(fc):
                    def t(slot):
                        return lambda e: e.dma_start(out=slot[:].rearrange("p (a b) -> p a b", b=2048), in_=wdv[fc])
                    return t
                utasks = [(P, kh) for P in range(NF // 2) for kh in range(2)]
                wsu = WStream(s, "wu", wu_sl, [mku(P, kh) for (P, kh) in utasks], lookahead=3, eng="gpsimd")
                wsd = WStream(s, "wd", wd_sl, [mkd(fc) for fc in range(NF)], lookahead=1, eng="gpsimd")
                uT_free = [None, None]
                rb_free = [None, None]
                ubanks = [[B[0], B[1]], [B[6], bank7f]]
                ub_free = [[None, None], [None, None]]
                mb_free = [None] * 4
                u_ready = {}
                cnt = {"nm": 0, "nr": 0}

                def do_up(fb):
                    ub = fb % 2
                    evs = []
                    if fb >= 1:
                        wsd.ensure((fb - 1) * 4 + 3)
                    for pp in range(2):
                        P = fb * 2 + pp
                        par = P % 2
                        banks = ubanks[par]
                        lm = None
                        for kh in range(2):
                            ti = P * 2 + kh
                            wt, wev = wsu.get(ti)
                            for ec in range(2):
                                for kc in range(16):
                                    first = (kh == 0 and kc == 0)
                                    last = (kh == 1 and kc == 15)
                                    lm = s.op("tensor", (lambda e, bank=banks[ec], kc=kc, kh=kh, ec=ec, wt=wt, first=first, last=last: e.matmul(bank[:], lhsT=wt[:, kc, ec * 128:(ec + 1) * 128], rhs=hT[:, kh * 16 + kc, :], start=first, stop=last)),
                                              waits=([wev] + ([ub_free[par][ec]] + hev if first else [])) if kc == 0 else (), sig=(kc == 15 and ec == 1))
                            wsu.release(ti, lm)
                        for ec in range(2):
                            j = pp * 2 + ec
                            r = cnt["nr"] % 2
                            cnt["nr"] += 1
                            e1 = s.op("scalar", (lambda e, r=r, bank=banks[ec]: e.activation(out=rb[r][:], in_=bank[:], func=AF.Relu)), waits=[lm, rb_free[r]])
                            ub_free[par][ec] = e1
                            e2 = s.op("scalar", (lambda e, r=r, ub=ub, j=j: e.activation(out=uT[ub][:, j, :], in_=rb[r][:], func=AF.Square)), waits=[e1, uT_free[ub]])
                            rb_free[r] = e2
                            evs.append(e2)
                    u_ready[fb] = evs

                def do_down(fb):
                    ub = fb % 2
                    wts = []
                    for j in range(4):
                        wt, wev = wsd.get(fb * 4 + j)
                        wts.append((wt, wev))
                    lm = None
                    ea = None
                    for dc in range(NC_):
                        mbk = cnt["nm"] % 4
                        cnt["nm"] += 1
                        bank = B[2 + mbk]
                        for j in range(4):
                            wt, wev = wts[j]
                            w_ = []
                            if dc == 0:
                                w_ += [wev, u_ready[fb][j]]
                            if j == 0:
                                w_.append(mb_free[mbk])
                            lm = s.op("tensor", (lambda e, bank=bank, wt=wt, dc=dc, j=j, ub=ub: e.matmul(bank[:], lhsT=wt[:, dc * 128:(dc + 1) * 128], rhs=uT[ub][:, j, :], start=(j == 0), stop=(j == 3))),
                                      waits=w_, sig=(j == 3))
                        if fb == 0:
                            ea = s.op("vector", (lambda e, bank=bank, dc=dc: e.tensor_copy(out=macc[:, dc, :], in_=bank[:])), waits=[lm])
                        else:
                            ea = s.op("vector", (lambda e, bank=bank, dc=dc: e.tensor_tensor(out=macc[:, dc, :], in0=macc[:, dc, :], in1=bank[:], op=ALU.add)), waits=[lm])
                        mb_free[mbk] = ea
                    for j in range(4):
                        wsd.release(fb * 4 + j, lm)
                    uT_free[ub] = lm
                    return ea

                do_up(0)
                last_acc = None
                for fb in range(NFB):
                    if fb + 1 < NFB:
                        do_up(fb + 1)
                    last_acc = do_down(fb)
                sq_free = [None, None]
                lmm = None
                for c in range(NC_):
                    i = c % 2
                    e1 = s.op("scalar", (lambda e, i=i, c=c: e.activation(out=sqm[i][:], in_=macc[:, c, :], func=AF.Square)), waits=[last_acc, sq_free[i]])
                    lmm = s.op("tensor", (lambda e, i=i, c=c: e.matmul(B[6][:], lhsT=self.ones[:], rhs=sqm[i][:], start=(c == 0), stop=(c == NC_ - 1))), waits=[e1])
                    sq_free[i] = lmm
                s.wait_only("scalar", [lmm])
                self.resid(l, 3, None, self.xa_d, out_d, [B[6]], tok0, HT, m_sb=macc)
            s.barrier()


def _consts(half):
    bf = ml_dtypes.bfloat16
    dt = np.empty((128, NT0 + NT1, 512), np.float32)
    i = np.arange(128)[:, None]
    j = np.arange(512)[None, :]
    idx = 0
    for qt in range(2):
        for kt in range(NT0 if qt == 0 else NT1):
            o = half * 1024 + qt * 512 - kt * 128
            dist = (o + j - i).astype(np.float32)
            dt[:, idx, :] = np.where(dist >= 0, -dist, NEGBIG)
            idx += 1
    p = np.arange(128)[:, None, None]
    q8 = np.arange(8)[None, :, None]
    n = np.arange(8)[None, None, :]
    pos = half * 1024 + q8 * 128 + p
    own = pos // 256
    pm = np.where(n < own, 0.0, -1.0e30).astype(np.float32).reshape(128, 64)
    oi = (n == own).astype(np.float32).reshape(128, 64)
    esel = np.zeros((8, 8, 128), np.float32)
    for r in range(8):
        esel[r, r, :] = 1.0
    return {
        "dtab": np.ascontiguousarray(dt.reshape(128, -1)),
        "pmask": pm, "ownind": oi,
        "ident": np.eye(128, dtype=np.float32).astype(bf),
        "onesb": np.ones((128, 128), np.float32).astype(bf),
        "onesf": np.ones((128, 128), np.float32),
        "esel": esel.reshape(8, 1024).astype(bf),
    }


_NC_CACHE = {}
_WSHAPE = {}


def _get_nc(n_layers=DEPTH, debug=False):
    key = (n_layers, debug)
    if key not in _NC_CACHE:
        b = Builder(n_layers, debug)
        _NC_CACHE[key] = b.build()
        _WSHAPE[key] = b.wshape
    return _NC_CACHE[key]


def _in_maps(inputs, wshape):
    x = np.asarray(inputs["x"], np.float32)
    f = lambda k: np.ascontiguousarray(np.asarray(inputs[k], np.float32))
    gl = []
    for name in ["g_pre_mix", "g_post_mix", "g_pre_mlp", "g_post_mlp"]:
        g = np.asarray(inputs[name], np.float32).reshape(DEPTH, NC_, 128)
        gl.append(g)
    gains = np.stack(gl, axis=1)
    gains = np.ascontiguousarray(gains.transpose(3, 0, 1, 2).reshape(128, -1))
    gsub = np.asarray(inputs["g_subln"], np.float32).reshape(DEPTH, 2, 128)
    gsub = np.ascontiguousarray(gsub.transpose(2, 0, 1).reshape(128, -1))
    lv = np.stack([np.asarray(inputs[k], np.float32) for k in ["lam_q1", "lam_k1", "lam_q2", "lam_k2"]], axis=1)
    lamv = np.ascontiguousarray(lv.transpose(2, 0, 1).reshape(128, -1))
    shared = {"gains": gains, "gsub": gsub, "lamv": lamv}
    for l in range(DEPTH):
        for nm in ["w_in", "w_proj_a", "w_proj_b", "w_out", "w_up", "w_down"]:
            shp = wshape[(nm, l)]
            w = np.asarray(inputs[nm][l], np.float32)
            shared[f"{nm}{l}"] = np.ascontiguousarray(w) if list(w.shape) == list(shp) else np.zeros(shp, np.float32)
    cs = [_consts(0), _consts(1)]
    maps = []
    for c in range(8):
        b, half = c // 2, c % 2
        xs = x[b, half * T:(half + 1) * T, :]
        xT = np.ascontiguousarray(xs.T).reshape(NC_, 128, T)
        m = {"xT": xT}
        m.update(shared)

        m.update(cs[half])
        maps.append(m)
    return maps


def kernel(**inputs):
    nc = _get_nc()
    maps = _in_maps(inputs, _WSHAPE[(DEPTH, False)])
    res = run_bass_kernel_spmd(nc, maps, core_ids=list(range(8)))
    out = np.empty((4, 2048, D), np.float32)
    for c in range(8):
        b, half = c // 2, c % 2
        oT = np.asarray(res.results[c]["outT"]).reshape(D, T)
        out[b, half * T:(half + 1) * T, :] = oT.T
    return out
```

```python
import contextlib
import math
import numpy as np
import ml_dtypes
import concourse.bass as bass
import concourse.mybir as mybir
from concourse.bass_utils import run_bass_kernel_spmd

F32 = mybir.dt.float32
BF16 = mybir.dt.bfloat16
ALU = mybir.AluOpType
AF = mybir.ActivationFunctionType
AX = mybir.AxisListType

D = 4096
NC_ = 32
T = 1024
DEPTH = 2
INW = 20480
DFF = 16384
EPS = 1e-6
SCALE = 128.0 ** -0.5
NEGBIG = -1.0e9
SELBIG = 30000.0
ENGINES = ["sync", "scalar", "gpsimd", "vector", "tensor"]
PIECES = {
    "winA": (4096, 8192),
    "winB": (4096, 12288),
    "wpa": (2048, 4096), "wpb": (2048, 4096), "wout": (4096, 4096),
    "wupA": (4096, 8192), "wupB": (4096, 8192),
    "wdnA": (8192, 4096), "wdnB": (8192, 4096),
}
NT0 = 12
NT1 = 16


class Ev:
    __slots__ = ("sem", "val")

    def __init__(self, sem, val):
        self.sem = sem
        self.val = val


class Sched:
    def __init__(self, nc):
        self.nc = nc
        self.q = {e: [] for e in ENGINES}
        self.sems = {}
        self.waited = {e: {} for e in ENGINES}
        self._ctx = []
        self.last = {}

    def sem(self, key):
        if key not in self.sems:
            cm = self.nc.semaphore("s_" + key)
            h = cm.__enter__()
            self._ctx.append(cm)
            self.sems[key] = [h, 0]
        return self.sems[key]

    def _waits(self, eng, waits):
        best = {}
        for ev in waits:
            if ev is None:
                continue
            if best.get(ev.sem, -1) < ev.val:
                best[ev.sem] = ev.val
        out = []
        w = self.waited[eng]
        for k, v in best.items():
            if w.get(k, -1) >= v:
                continue
            w[k] = v
            out.append(Ev(k, v))
        return out

    def op(self, eng, fn, waits=(), sig=True):
        ws = self._waits(eng, waits)
        ev = None
        if sig:
            s = self.sem("E_" + eng)
            s[1] += 1
            ev = Ev("E_" + eng, s[1])
            self.last[eng] = ev
        self.q[eng].append((fn, ws, ev, 1))
        return ev

    def dma(self, eng, fn, key, waits=(), inc=16):
        ws = self._waits(eng, waits)
        s = self.sem("D_" + key)
        s[1] += inc
        ev = Ev("D_" + key, s[1])
        self.q[eng].append((fn, ws, ev, inc))
        return ev

    def wait_only(self, eng, waits):
        ws = self._waits(eng, waits)
        if ws:
            self.q[eng].append((None, ws, None, 0))

    def all_events(self):
        evs = [ev for ev in self.last.values()]
        for k, v in self.sems.items():
            if k.startswith("D_") and v[1] > 0:
                evs.append(Ev(k, v[1]))
        return evs

    def barrier(self):
        evs = self.all_events()
        for e in ENGINES:
            self.wait_only(e, evs)

    def emit(self):
        nc = self.nc
        sems = self.sems
        with nc.Block() as block:
            def mk(engname):
                def body(e):
                    for fn, ws, ev, inc in self.q[engname]:
                        for w in ws:
                            e.wait_ge(sems[w.sem][0], w.val)
                        if fn is None:
                            continue
                        ins = fn(e)
                        if ev is not None:
                            if ev.sem.startswith("D_cc"):
                                ins.then_inc(sems[ev.sem][0])
                            else:
                                ins.then_inc(sems[ev.sem][0], inc)
                return body
            block.sync(mk("sync"))
            block.scalar(mk("scalar"))
            block.gpsimd(mk("gpsimd"))
            block.vector(mk("vector"))
            block.tensor(mk("tensor"))

    def close(self):
        for cm in reversed(self._ctx):
            cm.__exit__(None, None, None)


class WStream:
    def __init__(self, s, name, slots, tasks, lookahead, eng="sync", deps=None):
        self.s = s
        self.name = name
        self.slots = slots
        self.tasks = tasks
        self.look = min(lookahead, len(slots) - 1)
        self.emitted = 0
        self.loaded = {}
        self.free = [None] * len(slots)
        self.eng = eng
        self.deps = deps or (lambda j: [])

    def ensure(self, i):
        upto = min(len(self.tasks), i + self.look + 1)
        while self.emitted < upto:
            j = self.emitted
            sl = j % len(self.slots)
            fns = self.tasks[j](self.slots[sl])
            if not isinstance(fns, (list, tuple)):
                fns = [fns]
            for fn in fns:
                self.loaded[j] = self.s.dma(self.eng, fn, f"{self.name}{sl}", waits=[self.free[sl]] + list(self.deps(j)))
            self.emitted += 1

    def get(self, i):
        self.ensure(i)
        return self.slots[i % len(self.slots)], self.loaded[i]

    def release(self, i, ev):
        self.free[i % len(self.slots)] = ev


class Builder:
    def __init__(self, n_layers=DEPTH, debug=False, stop_after=99):
        self.stop_after = stop_after
        self.n_layers = n_layers
        self.debug = debug
        nc = bass.Bass("TRN2", target_bir_lowering=False)
        self.nc = nc
        self.s = Sched(nc)
        dk = "ExternalOutput" if debug else "Internal"

        def din(name, shape, dt):
            return nc.dram_tensor(name, shape, dt, kind="ExternalInput").ap()

        def dscr(name, shape, dt):
            return nc.dram_tensor(name, shape, dt, kind=dk).ap()

        self.x_in = din("xT", [NC_, 128, T], F32)
        WSH = {"w_in": [D, INW], "w_proj_a": [2048, D], "w_proj_b": [2048, D], "w_out": [D, D], "w_up": [D, DFF], "w_down": [DFF, D]}
        NEED = {"w_in": 1, "w_proj_a": 3, "w_proj_b": 3, "w_out": 3, "w_up": 4, "w_down": 4}
        self.W = {}
        self.wshape = {}
        for l in range(DEPTH):
            for nm, shp in WSH.items():
                needed = (l < n_layers - 1) or (l == n_layers - 1 and stop_after >= NEED[nm])
                shape = shp if needed else [8, 8]
                self.wshape[(nm, l)] = shape
                self.W[(nm, l)] = din(f"{nm}{l}", shape, F32)
        self.gains_d = din("gains", [128, DEPTH * 4 * NC_], F32)
        self.gsub_d = din("gsub", [128, DEPTH * 2], F32)
        self.lamv_d = din("lamv", [128, DEPTH * 4], F32)
        self.dtab_d = din("dtab", [128, (NT0 + NT1) * 512], F32)
        self.pm_d = din("pmask", [128, 64], F32)
        self.own_d = din("ownind", [128, 64], F32)
        self.ident_d = din("ident", [128, 128], BF16)
        self.ones_d = din("onesb", [128, 128], BF16)
        self.onesf_d = din("onesf", [128, 128], F32)
        self.esel_d = din("esel", [8, 8 * 128], BF16)
        self.out_d = nc.dram_tensor("outT", [NC_, 128, T], F32, kind="ExternalOutput").ap()

        self.xa_d = dscr("xa", [NC_, 128, T], F32)
        self.xb_d = dscr("xb", [NC_, 128, T], F32)
        self.q_d = dscr("qT", [NC_, 128, T], BF16)
        self.kv_loc = nc.dram_tensor("kv_loc", [8192, T], BF16).ap()
        self.kv_all = nc.dram_tensor("kv_all", [16384, T], BF16).ap()
        self.sg_d = dscr("sg", [64, 128, T], BF16)
        self.o_d = dscr("oT", [NC_, 128, T], BF16)
        self.mix_d = dscr("mix", [NC_, 128, T], F32)
        if debug:
            self.kvdbg_d = nc.dram_tensor("kvdbg", [16384, T], BF16, kind="ExternalOutput").ap()

    def sbuf(self, name, shape, dt):
        self._uid = getattr(self, "_uid", 0) + 1
        return self.nc.sbuf_tensor(f"{name}_u{self._uid}", shape, dt)

    def emit_casts(self):
        s = self.s
        self.cast_ev = {}
        first = [("winA", 0), ("winB", 0)]
        order = first + [(nm, l) for l in range(self.n_layers) for nm in PIECES if (nm, l) not in first]
        for (nm, l) in order:
            key = "cast0" if (nm, l) in first else "cast1"
            src_ = self.wsh[(nm, l)].rearrange("r (a b) -> r a b", b=2048)
            dst_ = self.wsb[(nm, l)].rearrange("r (a b) -> r a b", b=2048)
            self.cast_ev[(nm, l)] = s.dma("gpsimd", (lambda e, src_=src_, dst_=dst_: e.dma_start(out=dst_, in_=src_)), key)
        tot = s.sems["D_cast1"][1] if "D_cast1" in s.sems else 0
        for k in self.cast_ev:
            if k not in first:
                self.cast_ev[k] = Ev("D_cast1", tot)
        tot0 = s.sems["D_cast0"][1]
        for k in first:
            self.cast_ev[k] = Ev("D_cast0", tot0)

    def emit_gathers(self, keys):
        s = self.s
        for (nm, l) in keys:
            if l >= self.n_layers:
                continue
            a, b = self.wsb[(nm, l)], self.wfull[(nm, l)]
            self.wev[(nm, l)] = s.dma("gpsimd", (lambda e, a=a, b=b: e.collective_compute("AllGather", ALU.bypass, replica_groups=[list(range(8))], ins=[a], outs=[b])),
                                      f"ccw_{nm}{l}", waits=[self.cast_ev[(nm, l)]], inc=1)

    def build(self):
        nc, s = self.nc, self.s
        with contextlib.ExitStack() as gs:
            def sb(name, shape, dt):
                return gs.enter_context(self.sbuf(name, shape, dt))

            self.gains = sb("gains_s", [128, DEPTH * 4 * NC_], F32)
            self.gsub = sb("gsub_s", [128, DEPTH * 2], F32)
            self.lamv = sb("lamv_s", [128, DEPTH * 4], F32)
            self.ident = sb("ident_s", [128, 128], BF16)
            self.ones = sb("ones_s", [128, 128], BF16)
            self.onesf = sb("onesf_s", [128, 128], F32)
            self.esel = sb("esel_s", [8, 8 * 128], BF16)
            self.pm = sb("pm_s", [128, 64], F32)
            self.own = sb("own_s", [128, 64], F32)
            self.epsD = sb("epsD", [128, 1], F32)
            self.nlam = sb("nlam", [128, DEPTH], F32)
            self.gsl = sb("gsl", [128, DEPTH * 2], F32)
            self.lamt = sb("lamt", [128, 4], F32)
            self.banks = [gs.enter_context(nc.psum_tensor(f"bank{i}", [128, 512], F32)) for i in range(7)]
            self.bank7b = gs.enter_context(nc.psum_tensor("bank7", [128, 1024], BF16))

            cl = []
            for dst, src, k in [(self.gains, self.gains_d, "c0"), (self.gsub, self.gsub_d, "c1"),
                                (self.lamv, self.lamv_d, "c2"), (self.ident, self.ident_d, "c3"),
                                (self.ones, self.ones_d, "c4"), (self.onesf, self.onesf_d, "c5"),
                                (self.esel, self.esel_d, "c6"), (self.pm, self.pm_d, "c7"),
                                (self.own, self.own_d, "c8")]:
                cl.append(s.dma("sync", (lambda e, dst=dst, src=src: e.dma_start(out=dst[:], in_=src)), "c"))
            ev = s.op("vector", lambda e: e.memset(self.epsD[:], EPS))
            for l in range(self.n_layers):
                lam_init = 0.8 - 0.6 * math.exp(-0.3 * l)
                e1 = s.op("vector", lambda e, l=l: e.tensor_tensor(out=self.lamt[:, 0:1], in0=self.lamv[:, 4 * l:4 * l + 1], in1=self.lamv[:, 4 * l + 1:4 * l + 2], op=ALU.mult), waits=cl)
                e2 = s.op("vector", lambda e, l=l: e.tensor_tensor(out=self.lamt[:, 1:2], in0=self.lamv[:, 4 * l + 2:4 * l + 3], in1=self.lamv[:, 4 * l + 3:4 * l + 4], op=ALU.mult), waits=cl + [e1])
                e3 = s.op("tensor", lambda e: e.matmul(self.banks[0][:, 0:2], lhsT=self.onesf[:], rhs=self.lamt[:, 0:2], start=True, stop=True), waits=[e1, e2] + cl)
                e4 = s.op("scalar", lambda e: e.activation(out=self.lamt[:, 2:4], in_=self.banks[0][:, 0:2], func=AF.Exp), waits=[e3])
                e5 = s.op("vector", lambda e: e.tensor_tensor(out=self.lamt[:, 0:1], in0=self.lamt[:, 3:4], in1=self.lamt[:, 2:3], op=ALU.subtract), waits=[e4, e2])
                e6 = s.op("vector", lambda e, l=l, lam_init=lam_init: e.tensor_scalar(out=self.nlam[:, l:l + 1], in0=self.lamt[:, 0:1], scalar1=-lam_init, scalar2=None, op0=ALU.add), waits=[e5])
                e7 = s.op("vector", lambda e, l=l, lam_init=lam_init: e.tensor_scalar(out=self.gsl[:, 2 * l:2 * l + 2], in0=self.gsub[:, 2 * l:2 * l + 2], scalar1=(1.0 - lam_init), scalar2=None, op0=ALU.mult), waits=[e6])
                s.wait_only("tensor", [e5])
            s.barrier()

            x_cur = self.x_in
            for l in range(self.n_layers):
                last = (l == self.n_layers - 1)
                self.phase1(l, x_cur)
                s.barrier()
                if self.stop_after <= 1:
                    break
                self.phase2(l)
                s.barrier()
                if self.stop_after <= 2:
                    break
                self.phase3(l, x_cur)
                s.barrier()
                if self.stop_after <= 3:
                    break
                x_next = self.out_d if last else self.xb_d
                self.phase4(l, x_next)
                s.barrier()
                x_cur = x_next
            s.emit()
            s.close()
        return nc

    def prenorm(self, es, x_d, l, gidx, hT, tok0, ntok, ss_banks, tag):
        nc, s = self.nc, self.s
        ntg = ntok // 512
        NXS = 3
        xs = [es.enter_context(self.sbuf(f"xs{tag}{i}", [128, ntok], F32)) for i in range(NXS)]
        sqb = [es.enter_context(self.sbuf(f"sq{tag}{i}", [128, ntok], BF16)) for i in range(2)]
        rstd = es.enter_context(self.sbuf(f"rstd{tag}", [128, ntok], F32))
        xs_free = [None] * NXS
        sq_free = [None] * 2
        last_mm = None
        k = 0
        for c in range(NC_):
            sl = k % NXS
            ld = s.dma("sync", (lambda e, sl=sl, c=c: e.dma_start(out=xs[sl][:], in_=x_d[c, :, tok0:tok0 + ntok])), f"xs{sl}", waits=[xs_free[sl]])
            sq = s.op("scalar", (lambda e, sl=sl, c=c: e.activation(out=sqb[c % 2][:], in_=xs[sl][:], func=AF.Square)), waits=[ld, sq_free[c % 2]])
            for tg in range(ntg):
                last_mm = s.op("tensor", (lambda e, c=c, tg=tg: e.matmul(ss_banks[tg][:], lhsT=self.ones[:], rhs=sqb[c % 2][:, tg * 512:(tg + 1) * 512], start=(c == 0), stop=(c == NC_ - 1))), waits=[sq], sig=(tg == ntg - 1))
            xs_free[sl] = sq
            sq_free[c % 2] = last_mm
            k += 1
        ev = None
        for tg in range(ntg):
            e1 = s.op("scalar", (lambda e, tg=tg: e.activation(out=rstd[:, tg * 512:(tg + 1) * 512], in_=ss_banks[tg][:], func=AF.Ln, scale=1.0 / D, bias=self.epsD[:])), waits=[last_mm, ev])
            ev = s.op("scalar", (lambda e, tg=tg: e.activation(out=rstd[:, tg * 512:(tg + 1) * 512], in_=rstd[:, tg * 512:(tg + 1) * 512], func=AF.Exp, scale=-0.5)), waits=[e1])
        rs_ev = ev
        hev = []
        for c in range(NC_):
            sl = k % NXS
            ld = s.dma("sync", (lambda e, sl=sl, c=c: e.dma_start(out=xs[sl][:], in_=x_d[c, :, tok0:tok0 + ntok])), f"xs{sl}", waits=[xs_free[sl]])
            gcol = (l * 4 + gidx) * NC_ + c
            h = s.op("vector", (lambda e, sl=sl, c=c, gcol=gcol: e.scalar_tensor_tensor(out=hT[:, c, :], in0=xs[sl][:], scalar=self.gains[:, gcol:gcol + 1], in1=rstd[:], op0=ALU.mult, op1=ALU.mult)), waits=[ld, rs_ev])
            xs_free[sl] = h
            hev.append(h)
            k += 1
        return hev, last_mm

    def phase1(self, l, x_d):
        nc, s = self.nc, self.s
        B = self.banks
        with contextlib.ExitStack() as es:
            hT = es.enter_context(self.sbuf("hT1", [128, NC_, T], BF16))
            NS = 3
            wslots = [es.enter_context(self.sbuf(f"w1_{i}", [128, NC_, 512], BF16)) for i in range(NS)]
            NOB = 4
            ob = [es.enter_context(self.sbuf(f"ob1_{i}", [128, 512], BF16)) for i in range(NOB)]
            with contextlib.ExitStack() as es2:
                hev, _ = self.prenorm(es2, x_d, l, 0, hT, 0, T, [B[6], B[5]], "p1")
            ob_free = [None] * NOB
            order = list(range(40))
            NATCG = list(range(4, 8)) + list(range(16, 20)) + list(range(8, 12)) + list(range(20, 24)) + \
                list(range(0, 4)) + list(range(12, 16)) + list(range(24, 40))
            wv = self.W[("w_in", l)].rearrange("(kc p) c -> p kc c", p=128)

            def mk_task(cg):
                c0 = NATCG[cg] * 512

                def t(slot):
                    return [(lambda e, q=q: e.dma_start(out=slot[:, q * 8:(q + 1) * 8, :], in_=wv[:, q * 8:(q + 1) * 8, c0:c0 + 512])) for q in range(4)]
                return t
            ws = WStream(s, "w1s", wslots, [mk_task(cg) for cg in order], lookahead=2, eng="gpsimd")
            kvT = self.kv_loc[0:4096, :].rearrange("(j p) t -> j p t", p=128)
            vloc = self.kv_loc[4096:8192, :].rearrange("(t a) b -> t (a b)", a=4)
            pbank = [[B[0], B[1]], [B[2], B[3]]]
            pfree = [[None, None], [None, None]]
            kv_stores = []
            nout = 0
            cc_ev = None
            for i, cg in enumerate(order):
                wt, wev = ws.get(i)
                last_mm = None
                if 8 <= cg < 16:
                    vcol0 = (cg - 8) * 512
                    for tt in range(8):
                        pb = nout % 2
                        bank = pbank[pb][0]
                        for kc in range(NC_):
                            last_mm = s.op("tensor", (lambda e, bank=bank, kc=kc, tt=tt, wt=wt: e.matmul(bank[:], lhsT=hT[:, kc, tt * 128:(tt + 1) * 128], rhs=wt[:, kc, :], start=(kc == 0), stop=(kc == NC_ - 1))),
                                           waits=([wev, pfree[pb][0]] + hev) if kc == 0 else (), sig=(kc == NC_ - 1))
                        o = nout % NOB
                        eng = "vector" if nout % 2 == 0 else "scalar"
                        if eng == "vector":
                            ev = s.op("vector", (lambda e, o=o, bank=bank: e.tensor_copy(out=ob[o][:], in_=bank[:])), waits=[last_mm, ob_free[o]])
                        else:
                            ev = s.op("scalar", (lambda e, o=o, bank=bank: e.copy(out=ob[o][:], in_=bank[:])), waits=[last_mm, ob_free[o]])
                        pfree[pb][0] = ev
                        st = s.dma("sync", (lambda e, o=o, tt=tt, vcol0=vcol0: e.dma_start(out=vloc[tt * 128:(tt + 1) * 128, vcol0:vcol0 + 512], in_=ob[o][:])), f"ob1_{o}", waits=[ev])
                        ob_free[o] = st
                        kv_stores.append(st)
                        nout += 1
                else:
                    for ec in range(4):
                        ch = cg * 4 + ec
                        pb = nout % 2
                        for kc in range(NC_):
                            for tg in range(2):
                                bank = pbank[pb][tg]
                                last_mm = s.op("tensor", (lambda e, bank=bank, kc=kc, tg=tg, ec=ec, wt=wt: e.matmul(bank[:], lhsT=wt[:, kc, ec * 128:(ec + 1) * 128], rhs=hT[:, kc, tg * 512:(tg + 1) * 512], start=(kc == 0), stop=(kc == NC_ - 1))),
                                               waits=([wev, pfree[pb][tg]] + hev) if kc == 0 else (), sig=(kc == NC_ - 1 and tg == 1))
                        if ch < 32:
                            dst, sig_ = kvT[ch], False
                        elif ch < 96:
                            dst, sig_ = self.q_d[ch - 64], False
                        else:
                            dst, sig_ = self.sg_d[ch - 96], True
                        is_kv = ch < 32
                        for tg in range(2):
                            bank = pbank[pb][tg]
                            o = (nout * 2 + tg) % NOB
                            if sig_:
                                ev = s.op("scalar", (lambda e, o=o, bank=bank: e.activation(out=ob[o][:], in_=bank[:], func=AF.Sigmoid)), waits=[last_mm, ob_free[o]])
                            elif tg == 0:
                                ev = s.op("vector", (lambda e, o=o, bank=bank: e.tensor_copy(out=ob[o][:], in_=bank[:])), waits=[last_mm, ob_free[o]])
                            else:
                                ev = s.op("scalar", (lambda e, o=o, bank=bank: e.copy(out=ob[o][:], in_=bank[:])), waits=[last_mm, ob_free[o]])
                            pfree[pb][tg] = ev
                            st = s.dma("sync", (lambda e, o=o, dst=dst, tg=tg: e.dma_start(out=dst[:, tg * 512:(tg + 1) * 512], in_=ob[o][:])), f"ob1_{o}", waits=[ev])
                            ob_free[o] = st
                            if is_kv:
                                kv_stores.append(st)
                        nout += 1
                ws.release(i, last_mm)
                if i == 15:
                    for q in range(8):
                        cc_ev = s.dma("gpsimd", (lambda e, q=q: e.collective_compute("AllGather", ALU.bypass, replica_groups=[[0, 1], [2, 3], [4, 5], [6, 7]],
                                                                                    ins=[self.kv_loc[q * 1024:(q + 1) * 1024, :]], outs=[self.kv_all[q * 2048:(q + 1) * 2048, :]])), "cc", waits=kv_stores, inc=1)
            self.cc_ev = cc_ev
            if self.debug:
                for q in range(16):
                    s.dma("sync", (lambda e, q=q: e.dma_start(out=self.kvdbg_d[q * 1024:(q + 1) * 1024, :], in_=self.kv_all[q * 1024:(q + 1) * 1024, :])), "kvdbg", waits=[cc_ev])

    def phase2(self, l):
        nc, s = self.nc, self.s
        B = self.banks
        with contextlib.ExitStack() as es:
            dtab = es.enter_context(self.sbuf("dtab_s", [128, NT0 + NT1, 512], F32))
            ld_d = s.dma("sync", (lambda e: e.dma_start(out=dtab[:].rearrange("p a b -> p (a b)"), in_=self.dtab_d)), "dtab")
            KT = [es.enter_context(self.sbuf(f"KT{i}", [128, 2, T], BF16)) for i in range(2)]
            QT = [es.enter_context(self.sbuf(f"QT{i}", [128, T], BF16)) for i in range(2)]
            VT = [es.enter_context(self.sbuf(f"VT{i}", [128, 16, 256], BF16)) for i in range(2)]
            NST = 3
            st_ = [es.enter_context(self.sbuf(f"st{i}", [128, 512], F32)) for i in range(NST)]
            pt_ = [es.enter_context(self.sbuf(f"pt{i}", [128, 512], BF16)) for i in range(NST)]
            st_free = [None] * NST
            pt_free = [None] * NST
            rs_ = [es.enter_context(self.sbuf(f"rs{i}", [128, 512], F32)) for i in range(2)]
            oo_ = [es.enter_context(self.sbuf(f"oo{i}", [128, 512], BF16)) for i in range(4)]
            oo_free = [None] * 4
            km = es.enter_context(self.sbuf("km", [128, 8], F32))
            kmh = es.enter_context(self.sbuf("kmh", [128, 8], BF16))
            kmhf = es.enter_context(self.sbuf("kmhf", [128, 8], F32))
            kml = es.enter_context(self.sbuf("kml", [128, 8], BF16))
            gm = es.enter_context(self.sbuf("gm", [128, 64], F32))
            top8 = es.enter_context(self.sbuf("top8", [128, 64], F32))
            selb = es.enter_context(self.sbuf("selb", [128, 64], BF16))
            self_f = es.enter_context(self.sbuf("self_f", [128, 64], F32))
            selT = [es.enter_context(self.sbuf(f"selT{i}", [8, T], BF16)) for i in range(2)]
            A_ = [es.enter_context(self.sbuf(f"A{i}", [128, 2, 512], F32)) for i in range(2)]
            od = es.enter_context(self.sbuf("od", [128, 2, 512], F32))
            osq = es.enter_context(self.sbuf("osq", [128, 2, 512], BF16))
            rsub = es.enter_context(self.sbuf("rsub", [128, 512], F32))
            eps_s = es.enter_context(self.sbuf("eps_s", [128, 1], F32))
            s.op("vector", lambda e: e.memset(eps_s[:], EPS))
            bank7f = self.bank7b[:].bitcast(F32)

            cc = self.cc_ev
            ld_free = [None, None]
            nst = 0
            noo = 0

            def load_head(par, kchunk, qchunk, vcol0, vw):
                w = [ld_free[par], cc]
                evs = []
                kq, kr = kchunk // 8, (kchunk % 8) * 128
                for r in range(2):
                    r0 = kq * 2048 + r * 1024 + kr
                    evs.append(s.dma("sync", (lambda e, r=r, r0=r0: e.dma_start(out=KT[par][:, r, :], in_=self.kv_all[r0:r0 + 128, :])), f"kt{par}", waits=w))
                evs.append(s.dma("sync", (lambda e: e.dma_start(out=QT[par][:], in_=self.q_d[qchunk])), f"qt{par}", waits=w))
                for r in range(2):
                    for q4 in range(4):
                        r0 = (4 + q4) * 2048 + r * 1024
                        vsrc = self.kv_all[r0:r0 + 1024, :].rearrange("(t a) b -> t (a b)", a=4)
                        k0 = r * 8 + q4 * 2
                        evs.append(s.dma("sync", (lambda e, vsrc=vsrc, k0=k0: e.dma_start(out=VT[par][:, k0:k0 + 2, 0:vw], in_=vsrc[:, vcol0:vcol0 + vw].rearrange("(k p) d -> p k d", p=128))), f"vt{par}", waits=w))
                return evs

            def score_tiles(par, qt, c_h, s_banks, sel_par, on_pt):
                nonlocal nst
                nkt = NT0 if qt == 0 else NT1
                base = 0 if qt == 0 else NT0

                def emit_qk(kt):
                    sbk = s_banks[kt % 2]
                    r, kk = kt // 8, kt % 8
                    mm = s.op("tensor", (lambda e, sbk=sbk, r=r, kk=kk: e.matmul(sbk[:], lhsT=KT[par][:, r, kk * 128:(kk + 1) * 128], rhs=QT[par][:, qt * 512:(qt + 1) * 512], start=True, stop=(sel_par is None))),
                              waits=self._sfree[kt % 2:kt % 2 + 1] + self._ldev, sig=(sel_par is None))
                    if sel_par is not None:
                        n = kt // 2
                        mm = s.op("tensor", (lambda e, sbk=sbk, n=n: e.matmul(sbk[:], lhsT=self.esel[:, n * 128:(n + 1) * 128], rhs=selT[sel_par][:, qt * 512:(qt + 1) * 512], start=False, stop=True)), waits=self._selev)
                    return mm
                mms = {0: emit_qk(0)}
                for kt in range(nkt):
                    if kt + 1 < nkt:
                        mms[kt + 1] = emit_qk(kt + 1)
                    sbk = s_banks[kt % 2]
                    mm = mms[kt]
                    i = nst % NST
                    nst += 1
                    ea = s.op("vector", (lambda e, i=i, sbk=sbk, kt=kt: e.scalar_tensor_tensor(out=st_[i][:], in0=dtab[:, base + kt, :], scalar=float(c_h), in1=sbk[:], op0=ALU.mult, op1=ALU.add)), waits=[mm, st_free[i], ld_d])
                    self._sfree[kt % 2] = ea
                    ee = s.op("scalar", (lambda e, i=i: e.activation(out=pt_[i][:], in_=st_[i][:], func=AF.Exp, scale=SCALE)), waits=[ea, pt_free[i]])
                    st_free[i] = ee
                    evc = on_pt(kt, pt_[i], ee, kt == 0, kt == nkt - 1)
                    pt_free[i] = evc

            self._sfree = [None, None]
            heads = list(range(16))
            self._ldev = []
            nxt = load_head(0, 0, 0, 0 * 128, 128)
            acc_free = [[None, None], [None, None]]
            it = 0
            gate_free = None
            selT_free = [None, None]
            for hi, h in enumerate(heads):
                par = hi % 2
                ldev = nxt
                if hi + 1 < len(heads):
                    h2 = heads[hi + 1]
                    nxt = load_head(1 - par, h2, h2, h2 * 128, 128)
                slope = 2.0 ** (-8.0 * (h + 1) / 16.0)
                c_h = slope / SCALE
                e1 = s.op("vector", (lambda e, par=par: e.tensor_reduce(out=km[:], in_=KT[par][:].rearrange("p r (n k) -> p (r n) k", k=256), axis=AX.X, op=ALU.add)), waits=ldev + [gate_free])
                e2 = s.op("vector", lambda e: e.tensor_scalar(out=km[:], in0=km[:], scalar1=1.0 / 256.0, scalar2=None, op0=ALU.mult), waits=[e1])
                e3 = s.op("vector", lambda e: e.tensor_copy(out=kmh[:], in_=km[:]), waits=[e2])
                e4 = s.op("vector", lambda e: e.tensor_copy(out=kmhf[:], in_=kmh[:]), waits=[e3])
                e5 = s.op("vector", lambda e: e.tensor_tensor(out=kml[:], in0=km[:], in1=kmhf[:], op=ALU.subtract), waits=[e4])
                gmm = None
                for q8 in range(8):
                    s.op("tensor", (lambda e, q8=q8, par=par: e.matmul(B[6][:, q8 * 8:(q8 + 1) * 8], lhsT=QT[par][:, q8 * 128:(q8 + 1) * 128], rhs=kmh[:], start=True, stop=False)), waits=[e5, gate_free] + ldev, sig=False)
                    gmm = s.op("tensor", (lambda e, q8=q8, par=par: e.matmul(B[6][:, q8 * 8:(q8 + 1) * 8], lhsT=QT[par][:, q8 * 128:(q8 + 1) * 128], rhs=kml[:], start=False, stop=True)), sig=(q8 == 7))
                e6 = s.op("vector", lambda e: e.tensor_tensor(out=gm[:], in0=B[6][:, 0:64], in1=self.pm[:], op=ALU.add), waits=[gmm])
                gate_free = e6
                ev = e6
                for q8 in range(8):
                    ev = s.op("vector", (lambda e, q8=q8: e.max(out=top8[:, q8 * 8:(q8 + 1) * 8], in_=gm[:, q8 * 8:(q8 + 1) * 8])), waits=[ev])
                for q8 in range(8):
                    ev = s.op("vector", (lambda e, q8=q8: e.tensor_scalar(out=self_f[:, q8 * 8:(q8 + 1) * 8], in0=gm[:, q8 * 8:(q8 + 1) * 8], scalar1=top8[:, q8 * 8 + 2:q8 * 8 + 3], scalar2=None, op0=ALU.is_ge)), waits=[ev])
                ev = s.op("vector", lambda e: e.tensor_tensor(out=self_f[:], in0=self_f[:], in1=self.own[:], op=ALU.max), waits=[ev])
                ev = s.op("vector", lambda e: e.tensor_scalar(out=selb[:], in0=self_f[:], scalar1=-1.0, scalar2=SELBIG, op0=ALU.add, op1=ALU.mult), waits=[ev, self._b7free])
                tr = None
                for q8 in range(8):
                    tr = s.op("tensor", (lambda e, q8=q8: e.transpose(out=self.bank7b[0:8, q8 * 128:(q8 + 1) * 128], in_=selb[:, q8 * 8:(q8 + 1) * 8], identity=self.ident[:])), waits=[ev, self._b7free] if q8 == 0 else (), sig=(q8 == 7))
                evs = s.op("vector", (lambda e, par=par: e.tensor_copy(out=selT[par][:], in_=self.bank7b[0:8, :])), waits=[tr, selT_free[par]])
                self._b7free = evs
                self._selev = [evs]
                self._ldev = ldev
                last_use = None
                for qt in range(2):
                    ps = it % 2
                    it += 1
                    Ob, Sb = B[2 + ps], B[4 + ps]

                    def on_pt(kt, pt, ee, first, lastk, Ob=Ob, Sb=Sb, par=par, ps=ps):
                        s.op("tensor", (lambda e: e.matmul(Ob[:], lhsT=VT[par][:, kt, 0:128], rhs=pt[:], start=first, stop=lastk)), waits=[ee] + ([acc_free[ps][0]] if first else []), sig=False)
                        return s.op("tensor", (lambda e: e.matmul(Sb[:], lhsT=self.ones[:], rhs=pt[:], start=first, stop=lastk)), waits=[acc_free[ps][1]] if first else ())
                    self._last_pv = None

                    def on_pt2(kt, pt, ee, first, lastk):
                        ev = on_pt(kt, pt, ee, first, lastk)
                        self._last_pv = ev
                        return ev
                    score_tiles(par, qt, c_h, [B[0], B[1]], par, on_pt2)
                    lp = self._last_pv
                    r = it % 2
                    e1 = s.op("vector", (lambda e, r=r, Sb=Sb: e.reciprocal(out=rs_[r][:], in_=Sb[:])), waits=[lp])
                    o = noo % 4
                    noo += 1
                    e2 = s.op("vector", (lambda e, r=r, o=o, Ob=Ob: e.tensor_tensor(out=oo_[o][:], in0=Ob[:], in1=rs_[r][:], op=ALU.mult)), waits=[e1, oo_free[o]])
                    acc_free[ps] = [e2, e1]
                    st = s.dma("sync", (lambda e, o=o, h=h, qt=qt: e.dma_start(out=self.o_d[h][:, qt * 512:(qt + 1) * 512], in_=oo_[o][:])), f"oo{o}", waits=[e2])
                    oo_free[o] = st
                    last_use = lp
                selT_free[par] = last_use
                ld_free[par] = last_use
            s.barrier()
            ld_free = [None, None]
            self._selev = []
            nld = 0
            seq = [(h, m) for h in range(8) for m in range(2)]
            nxt = load_head(0, 16 + 0, 16 + 0, 2048, 256)
            it = 0
            acc_free = [[None, None, None], [None, None, None]]
            A_free = [None, None]
            od_free = None
            for si, (h, m) in enumerate(seq):
                par = si % 2
                ldev = nxt
                if si + 1 < len(seq):
                    h2, m2 = seq[si + 1]
                    nxt = load_head(1 - par, 16 + 2 * h2 + m2, 16 + 2 * h2 + m2, 2048 + h2 * 256, 256)
                slope = 2.0 ** (-8.0 * (h + 1) / 8.0)
                c_h = slope / SCALE
                self._ldev = ldev
                last_use = None
                for qt in range(2):
                    ps = it % 2
                    it += 1
                    O0, O1, Sb = (B[2], B[3], B[4]) if ps == 0 else (B[5], B[6], bank7f)

                    def on_pt(kt, pt, ee, first, lastk, O0=O0, O1=O1, Sb=Sb, par=par, ps=ps):
                        s.op("tensor", (lambda e: e.matmul(O0[:], lhsT=VT[par][:, kt, 0:128], rhs=pt[:], start=first, stop=lastk)), waits=[ee] + ([acc_free[ps][0]] if first else []), sig=False)
                        s.op("tensor", (lambda e: e.matmul(O1[:], lhsT=VT[par][:, kt, 128:256], rhs=pt[:], start=first, stop=lastk)), waits=[acc_free[ps][1]] if first else (), sig=False)
                        ev = s.op("tensor", (lambda e: e.matmul(Sb[:], lhsT=self.ones[:], rhs=pt[:], start=first, stop=lastk)), waits=[acc_free[ps][2]] if first else ())
                        self._last_pv = ev
                        return ev
                    score_tiles(par, qt, c_h, [B[0], B[1]], None, on_pt)
                    lp = self._last_pv
                    r = it % 2
                    e1 = s.op("vector", (lambda e, r=r, Sb=Sb: e.reciprocal(out=rs_[r][:], in_=Sb[:])), waits=[lp])
                    a = A_[qt]
                    if m == 0:
                        e2 = s.op("vector", (lambda e, r=r, a=a, O0=O0: e.tensor_tensor(out=a[:, 0, :], in0=O0[:], in1=rs_[r][:], op=ALU.mult)), waits=[e1, A_free[qt]])
                        e3 = s.op("vector", (lambda e, r=r, a=a, O1=O1: e.tensor_tensor(out=a[:, 1, :], in0=O1[:], in1=rs_[r][:], op=ALU.mult)), waits=[e1, A_free[qt]])
                        acc_free[ps] = [e2, e3, e1]
                    else:
                        e2 = s.op("vector", (lambda e, r=r, O0=O0: e.tensor_tensor(out=od[:, 0, :], in0=O0[:], in1=rs_[r][:], op=ALU.mult)), waits=[e1, od_free])
                        e3 = s.op("vector", (lambda e, r=r, O1=O1: e.tensor_tensor(out=od[:, 1, :], in0=O1[:], in1=rs_[r][:], op=ALU.mult)), waits=[e1, od_free])
                        acc_free[ps] = [e2, e3, e1]
                        e4 = s.op("vector", (lambda e, a=a: e.scalar_tensor_tensor(out=od[:].rearrange("p a b -> p (a b)"), in0=od[:].rearrange("p a b -> p (a b)"), scalar=self.nlam[:, l:l + 1], in1=a[:].rearrange("p a b -> p (a b)"), op0=ALU.mult, op1=ALU.add)), waits=[e2, e3])
                        A_free[qt] = e4
                        e5 = s.op("scalar", (lambda e: e.activation(out=osq[:].rearrange("p a b -> p (a b)"), in_=od[:].rearrange("p a b -> p (a b)"), func=AF.Square)), waits=[e4, self._osq_free])
                        s.op("tensor", (lambda e, Sb=Sb: e.matmul(Sb[:], lhsT=self.ones[:], rhs=osq[:, 0, :], start=True, stop=False)), waits=[e5, e1], sig=False)
                        e6 = s.op("tensor", (lambda e, Sb=Sb: e.matmul(Sb[:], lhsT=self.ones[:], rhs=osq[:, 1, :], start=False, stop=True)))
                        self._osq_free = e6
                        e7 = s.op("scalar", (lambda e, Sb=Sb: e.activation(out=rsub[:], in_=Sb[:], func=AF.Ln, scale=1.0 / 256.0, bias=eps_s[:])), waits=[e6, self._rsub_free])
                        e8 = s.op("scalar", (lambda e: e.activation(out=rsub[:], in_=rsub[:], func=AF.Exp, scale=-0.5)), waits=[e7])
                        acc_free[ps][2] = e7
                        evo = None
                        for c in range(2):
                            o = noo % 4
                            noo += 1
                            e9 = s.op("vector", (lambda e, c=c, o=o: e.scalar_tensor_tensor(out=oo_[o][:], in0=od[:, c, :], scalar=self.gsl[:, 2 * l + c:2 * l + c + 1], in1=rsub[:], op0=ALU.mult, op1=ALU.mult)), waits=[e8, e4, oo_free[o]])
                            st = s.dma("sync", (lambda e, o=o, h=h, c=c, qt=qt: e.dma_start(out=self.o_d[16 + 2 * h + c][:, qt * 512:(qt + 1) * 512], in_=oo_[o][:])), f"oo{o}", waits=[e9])
                            oo_free[o] = st
                            evo = e9
                        od_free = evo
                        self._rsub_free = evo
                    last_use = lp
                ld_free[par] = last_use

    _b7free = None
    _osq_free = None
    _rsub_free = None

    def phase3(self, l, x_d):
        nc, s = self.nc, self.s
        B = self.banks
        bank7f = self.bank7b[:].bitcast(F32)
        with contextlib.ExitStack() as es:
            yT = es.enter_context(self.sbuf("yT", [128, NC_, T], BF16))
            yev = []
            with contextlib.ExitStack() as es2:
                oT = es2.enter_context(self.sbuf("oT", [128, NC_, T], BF16))
                lo = []
                for c in range(NC_):
                    lo.append(s.dma("sync", (lambda e, c=c: e.dma_start(out=oT[:, c, :], in_=self.o_d[c])), f"oT{c % 4}"))
                wa_sl = [es2.enter_context(self.sbuf(f"wa{i}", [128, 16, 256], BF16)) for i in range(3)]
                wb_sl = [es2.enter_context(self.sbuf(f"wb{i}", [128, 16, 256], BF16)) for i in range(3)]
                wav = self.W[("w_proj_a", l)].rearrange("(kc p) c -> p kc c", p=128)
                wbv = self.W[("w_proj_b", l)].rearrange("(kc p) c -> p kc c", p=128)

                def mk(wv_, d2):
                    def t(slot):
                        return lambda e: e.dma_start(out=slot[:], in_=wv_[:, :, d2 * 256:(d2 + 1) * 256])
                    return t
                wsa = WStream(s, "wpa", wa_sl, [mk(wav, d2) for d2 in range(NC_ // 2)], lookahead=2, eng="gpsimd")
                wsb = WStream(s, "wpb", wb_sl, [mk(wbv, d2) for d2 in range(NC_ // 2)], lookahead=2, eng="gpsimd")
                sga = [es2.enter_context(self.sbuf(f"sga{i}", [128, T], BF16)) for i in range(2)]
                sgb = [es2.enter_context(self.sbuf(f"sgb{i}", [128, T], BF16)) for i in range(2)]
                t1 = [es2.enter_context(self.sbuf(f"t1_{i}", [128, 512], F32)) for i in range(2)]
                t2 = [es2.enter_context(self.sbuf(f"t2_{i}", [128, 512], F32)) for i in range(2)]
                sg_free = [None, None]
                t_free = [None, None]
                pfree = [[None] * 4, [None] * 4]
                nt = 0
                for dc in range(NC_):
                    pb = dc % 2
                    banks = [B[0], B[1], B[2], B[3]] if pb == 0 else [B[4], B[5], B[6], bank7f]
                    wa, eva = wsa.get(dc // 2)
                    wb, evb = wsb.get(dc // 2)
                    wc0 = (dc % 2) * 128
                    g = dc % 2
                    lga = s.dma("sync", (lambda e, g=g, dc=dc: e.dma_start(out=sga[g][:], in_=self.sg_d[dc])), f"sga{g}", waits=[sg_free[g]])
                    lgb = s.dma("sync", (lambda e, g=g, dc=dc: e.dma_start(out=sgb[g][:], in_=self.sg_d[32 + dc])), f"sgb{g}", waits=[sg_free[g]])
                    lma = lmb = None
                    for kc in range(16):
                        for tg in range(2):
                            lma = s.op("tensor", (lambda e, bk=banks[tg], kc=kc, tg=tg, wa=wa, wc0=wc0: e.matmul(bk[:], lhsT=wa[:, kc, wc0:wc0 + 128], rhs=oT[:, kc, tg * 512:(tg + 1) * 512], start=(kc == 0), stop=(kc == 15))),
                                       waits=([eva, pfree[pb][tg]] + lo) if kc == 0 else (), sig=(kc == 15 and tg == 1))
                    for kc in range(16):
                        for tg in range(2):
                            lmb = s.op("tensor", (lambda e, bk=banks[2 + tg], kc=kc, tg=tg, wb=wb, wc0=wc0: e.matmul(bk[:], lhsT=wb[:, kc, wc0:wc0 + 128], rhs=oT[:, 16 + kc, tg * 512:(tg + 1) * 512], start=(kc == 0), stop=(kc == 15))),
                                       waits=([evb, pfree[pb][2 + tg]]) if kc == 0 else (), sig=(kc == 15 and tg == 1))
                    if dc % 2 == 1:
                        wsa.release(dc // 2, lma)
                        wsb.release(dc // 2, lmb)
                    last = None
                    for tg in range(2):
                        ti = nt % 2
                        nt += 1
                        e1 = s.op("vector", (lambda e, ti=ti, bk=banks[tg], g=g, tg=tg: e.tensor_tensor(out=t1[ti][:], in0=bk[:], in1=sga[g][:, tg * 512:(tg + 1) * 512], op=ALU.mult)), waits=[lma, lga, t_free[ti]])
                        e2 = s.op("vector", (lambda e, ti=ti, bk=banks[2 + tg], g=g, tg=tg: e.tensor_tensor(out=t2[ti][:], in0=bk[:], in1=sgb[g][:, tg * 512:(tg + 1) * 512], op=ALU.mult)), waits=[lmb, lgb, t_free[ti]])
                        pfree[pb][tg] = e1
                        pfree[pb][2 + tg] = e2
                        e3 = s.op("vector", (lambda e, ti=ti, dc=dc, tg=tg: e.tensor_tensor(out=yT[:, dc, tg * 512:(tg + 1) * 512], in0=t1[ti][:], in1=t2[ti][:], op=ALU.add)), waits=[e1, e2])
                        t_free[ti] = e3
                        yev.append(e3)
                        last = e2
                    sg_free[g] = last
            s.barrier()
            with contextlib.ExitStack() as es3:
                wo_sl = [es3.enter_context(self.sbuf(f"wo{i}", [128, NC_, 256], BF16)) for i in range(3)]
                wov = self.W[("w_out", l)].rearrange("(kc p) c -> p kc c", p=128)

                def mko(d2):
                    def t(slot):
                        return [(lambda e, q=q: e.dma_start(out=slot[:, q * 16:(q + 1) * 16, :], in_=wov[:, q * 16:(q + 1) * 16, d2 * 256:(d2 + 1) * 256])) for q in range(2)]
                    return t
                wso = WStream(s, "wo", wo_sl, [mko(d2) for d2 in range(NC_ // 2)], lookahead=2, eng="gpsimd")
                stg = [es3.enter_context(self.sbuf(f"mst{i}", [128, 512], F32)) for i in range(4)]
                sq = [es3.enter_context(self.sbuf(f"msq{i}", [128, 512], BF16)) for i in range(4)]
                stg_free = [None] * 4
                self._stg_rd = [None] * 4
                sq_free = [None] * 4
                pfree = [[None, None], [None, None]]
                n = 0
                ss_last = None
                ss_pend = []
                for dc in range(NC_):
                    pb = dc % 2
                    banks = [B[0], B[1]] if pb == 0 else [B[2], B[3]]
                    wo, evo = wso.get(dc // 2)
                    wc0 = (dc % 2) * 128
                    lm = None
                    for kc in range(NC_):
                        for tg in range(2):
                            lm = s.op("tensor", (lambda e, bk=banks[tg], kc=kc, tg=tg, wo=wo, wc0=wc0: e.matmul(bk[:], lhsT=wo[:, kc, wc0:wc0 + 128], rhs=yT[:, kc, tg * 512:(tg + 1) * 512], start=(kc == 0), stop=(kc == NC_ - 1))),
                                      waits=([evo, pfree[pb][tg]] + yev) if kc == 0 else (), sig=(kc == NC_ - 1 and tg == 1))
                    if dc % 2 == 1:
                        wso.release(dc // 2, lm)
                    for fn_ in ss_pend:
                        fn_()
                    ss_pend = []
                    for tg in range(2):
                        i = n % 4
                        n += 1
                        e1 = s.op("vector", (lambda e, i=i, bk=banks[tg]: e.tensor_copy(out=stg[i][:], in_=bk[:])), waits=[lm, stg_free[i], self._stg_rd[i]])
                        pfree[pb][tg] = e1
                        e2 = s.op("scalar", (lambda e, i=i: e.activation(out=sq[i][:], in_=stg[i][:], func=AF.Square)), waits=[e1, sq_free[i]])
                        st = s.dma("sync", (lambda e, i=i, dc=dc, tg=tg: e.dma_start(out=self.mix_d[dc][:, tg * 512:(tg + 1) * 512], in_=stg[i][:])), f"mst{i}", waits=[e1])
                        stg_free[i] = st
                        self._stg_rd[i] = e2
                        ssb = B[5] if tg == 0 else B[6]

                        def ss_fn(i=i, ssb=ssb, dc=dc, e2=e2):
                            ev = s.op("tensor", (lambda e: e.matmul(ssb[:], lhsT=self.ones[:], rhs=sq[i][:], start=(dc == 0), stop=(dc == NC_ - 1))), waits=[e2])
                            sq_free[i] = ev
                        ss_pend.append(ss_fn)
                for fn_ in ss_pend:
                    fn_()
                ss_pend = []
            s.barrier()
            self.resid(l, 1, self.mix_d, x_d, self.xa_d, [B[5], B[6]], 0, T)

    def resid(self, l, gidx, m_d, x_d, out_d, ss_banks, tok0, ntok, m_sb=None):
        nc, s = self.nc, self.s
        ntg = ntok // 512
        with contextlib.ExitStack() as es:
            rstd = es.enter_context(self.sbuf("rstd_r", [128, ntok], F32))
            ev = None
            for tg in range(ntg):
                e1 = s.op("scalar", (lambda e, tg=tg: e.activation(out=rstd[:, tg * 512:(tg + 1) * 512], in_=ss_banks[tg][:], func=AF.Ln, scale=1.0 / D, bias=self.epsD[:])), waits=[ev])
                ev = s.op("scalar", (lambda e, tg=tg: e.activation(out=rstd[:, tg * 512:(tg + 1) * 512], in_=rstd[:, tg * 512:(tg + 1) * 512], func=AF.Exp, scale=-0.5)), waits=[e1])
            rs_ev = ev
            NB = 3
            mb = [es.enter_context(self.sbuf(f"rm{i}", [128, ntok], F32)) for i in range(NB)] if m_sb is None else None
            xb = [es.enter_context(self.sbuf(f"rx{i}", [128, ntok], F32)) for i in range(NB)]
            m_free = [None] * NB
            x_free = [None] * NB
            for c in range(NC_):
                i = c % NB
                waits = [rs_ev]
                if m_sb is None:
                    lm = s.dma("sync", (lambda e, i=i, c=c: e.dma_start(out=mb[i][:], in_=m_d[c][:, tok0:tok0 + ntok])), f"rm{i}", waits=[m_free[i]])
                    msrc = mb[i][:]
                    waits.append(lm)
                else:
                    msrc = m_sb[:, c, :]
                lx = s.dma("sync", (lambda e, i=i, c=c: e.dma_start(out=xb[i][:], in_=x_d[c][:, tok0:tok0 + ntok])), f"rx{i}", waits=[x_free[i]])
                gcol = (l * 4 + gidx) * NC_ + c
                if m_sb is None:
                    e1 = s.op("vector", (lambda e, i=i, gcol=gcol, msrc=msrc: e.scalar_tensor_tensor(out=mb[i][:], in0=msrc, scalar=self.gains[:, gcol:gcol + 1], in1=rstd[:], op0=ALU.mult, op1=ALU.mult)), waits=waits)
                    e2 = s.op("vector", (lambda e, i=i: e.tensor_tensor(out=xb[i][:], in0=xb[i][:], in1=mb[i][:], op=ALU.add)), waits=[e1, lx])
                    m_free[i] = e2
                else:
                    e1 = s.op("vector", (lambda e, c=c, gcol=gcol, msrc=msrc: e.scalar_tensor_tensor(out=msrc, in0=msrc, scalar=self.gains[:, gcol:gcol + 1], in1=rstd[:], op0=ALU.mult, op1=ALU.mult)), waits=waits)
                    e2 = s.op("vector", (lambda e, i=i, msrc=msrc: e.tensor_tensor(out=xb[i][:], in0=xb[i][:], in1=msrc, op=ALU.add)), waits=[e1, lx])
                st = s.dma("sync", (lambda e, i=i, c=c: e.dma_start(out=out_d[c][:, tok0:tok0 + ntok], in_=xb[i][:])), f"rxs{i}", waits=[e2])
                x_free[i] = st

    def phase4(self, l, out_d):
        nc, s = self.nc, self.s
        B = self.banks
        bank7f = self.bank7b[:].bitcast(F32)
        HT = 512
        NFB = DFF // 512
        NF = DFF // 128
        for hh in range(2):
            tok0 = hh * HT
            with contextlib.ExitStack() as es:
                hT = es.enter_context(self.sbuf("hT4", [128, NC_, HT], BF16))
                macc = es.enter_context(self.sbuf("macc", [128, NC_, HT], F32))
                wu_sl = [es.enter_context(self.sbuf(f"wu{i}", [128, 16, 256], BF16)) for i in range(4)]
                wd_sl = [es.enter_context(self.sbuf(f"wd{i}", [128, 2048], BF16)) for i in range(10)]
                uT = [es.enter_context(self.sbuf(f"uT{i}", [128, 4, HT], BF16)) for i in range(2)]
                rb = [es.enter_context(self.sbuf(f"rb{i}", [128, HT], F32)) for i in range(2)]
                sqm = [es.enter_context(self.sbuf(f"sqm{i}", [128, HT], BF16)) for i in range(2)]
                with contextlib.ExitStack() as es2:
                    hev, _ = self.prenorm(es2, self.xa_d, l, 2, hT, tok0, HT, [B[6]], "p4")
                wuv = self.W[("w_up", l)].rearrange("(kh kc p) c -> kh p kc c", p=128, kh=2)
                wdv = self.W[("w_down", l)].rearrange("(fc p) (a b) -> fc p a b", p=128, b=2048)

                def mku(P, kh):
                    def t(slot):
                        return lambda e: e.dma_start(out=slot[:], in_=wuv[kh][:, :, P * 256:(P + 1) * 256])
                    return t

                def mkd(fc, half):
                    def t(slot):
                        return lambda e: e.dma_start(out=slot[:], in_=wdv[fc][:, half, :])
                    return t
                dtasks = [(fb * 4 + j, half) for fb in range(NFB) for half in range(2) for j in range(4)]
                utasks = [(P, kh) for P in range(NF // 2) for kh in range(2)]
                wsu = WStream(s, "wu", wu_sl, [mku(P, kh) for (P, kh) in utasks], lookahead=3, eng="gpsimd")
                wsd = WStream(s, "wd", wd_sl, [mkd(fc, half) for (fc, half) in dtasks], lookahead=6, eng="gpsimd")
                uT_free = [None, None]
                rb_free = [None, None]
                ubanks = [[B[0], B[1]], [B[6], bank7f]]
                ub_free = [[None, None], [None, None]]
                mb_free = [None] * 4
                u_ready = {}
                cnt = {"nm": 0, "nr": 0}

                def do_up(fb):
                    ub = fb % 2
                    evs = []
                    for pp in range(2):
                        P = fb * 2 + pp
                        par = P % 2
                        banks = ubanks[par]
                        lm = None
                        for kh in range(2):
                            ti = P * 2 + kh
                            wt, wev = wsu.get(ti)
                            for ec in range(2):
                                for kc in range(16):
                                    first = (kh == 0 and kc == 0)
                                    last = (kh == 1 and kc == 15)
                                    lm = s.op("tensor", (lambda e, bank=banks[ec], kc=kc, kh=kh, ec=ec, wt=wt, first=first, last=last: e.matmul(bank[:], lhsT=wt[:, kc, ec * 128:(ec + 1) * 128], rhs=hT[:, kh * 16 + kc, :], start=first, stop=last)),
                                              waits=([wev] + ([ub_free[par][ec]] + hev if first else [])) if kc == 0 else (), sig=(kc == 15 and ec == 1))
                            wsu.release(ti, lm)
                        for ec in range(2):
                            j = pp * 2 + ec
                            r = cnt["nr"] % 2
                            cnt["nr"] += 1
                            e1 = s.op("scalar", (lambda e, r=r, bank=banks[ec]: e.activation(out=rb[r][:], in_=bank[:], func=AF.Relu)), waits=[lm, rb_free[r]])
                            ub_free[par][ec] = e1
                            e2 = s.op("scalar", (lambda e, r=r, ub=ub, j=j: e.activation(out=uT[ub][:, j, :], in_=rb[r][:], func=AF.Square)), waits=[e1, uT_free[ub]])
                            rb_free[r] = e2
                            evs.append(e2)
                    u_ready[fb] = evs

                def do_down(fb):
                    ub = fb % 2
                    lm = None
                    ea = None
                    for half in range(2):
                        wts = [wsd.get(fb * 8 + half * 4 + j) for j in range(4)]
                        for dcl in range(16):
                            dc = half * 16 + dcl
                            mbk = cnt["nm"] % 4
                            cnt["nm"] += 1
                            bank = B[2 + mbk]
                            for j in range(4):
                                wt, wev = wts[j]
                                w_ = []
                                if dcl == 0:
                                    w_ += [wev, u_ready[fb][j]]
                                if j == 0:
                                    w_.append(mb_free[mbk])
                                lm = s.op("tensor", (lambda e, bank=bank, wt=wt, dcl=dcl, j=j, ub=ub: e.matmul(bank[:], lhsT=wt[:, dcl * 128:(dcl + 1) * 128], rhs=uT[ub][:, j, :], start=(j == 0), stop=(j == 3))),
                                          waits=w_, sig=(j == 3))
                            if fb == 0:
                                ea = s.op("vector", (lambda e, bank=bank, dc=dc: e.tensor_copy(out=macc[:, dc, :], in_=bank[:])), waits=[lm])
                            else:
                                ea = s.op("vector", (lambda e, bank=bank, dc=dc: e.tensor_tensor(out=macc[:, dc, :], in0=macc[:, dc, :], in1=bank[:], op=ALU.add)), waits=[lm])
                            mb_free[mbk] = ea
                        for j in range(4):
                            wsd.release(fb * 8 + half * 4 + j, lm)
                    uT_free[ub] = lm
                    return ea

                do_up(0)
                last_acc = None
                for fb in range(NFB):
                    if fb + 1 < NFB:
                        do_up(fb + 1)
                    last_acc = do_down(fb)
                sq_free = [None, None]
                lmm = None
                for c in range(NC_):
                    i = c % 2
                    e1 = s.op("scalar", (lambda e, i=i, c=c: e.activation(out=sqm[i][:], in_=macc[:, c, :], func=AF.Square)), waits=[last_acc, sq_free[i]])
                    lmm = s.op("tensor", (lambda e, i=i, c=c: e.matmul(B[6][:], lhsT=self.ones[:], rhs=sqm[i][:], start=(c == 0), stop=(c == NC_ - 1))), waits=[e1])
                    sq_free[i] = lmm
                s.wait_only("scalar", [lmm])
                self.resid(l, 3, None, self.xa_d, out_d, [B[6]], tok0, HT, m_sb=macc)
            s.barrier()


def _consts(half):
    bf = ml_dtypes.bfloat16
    dt = np.empty((128, NT0 + NT1, 512), np.float32)
    i = np.arange(128)[:, None]
    j = np.arange(512)[None, :]
    idx = 0
    for qt in range(2):
        for kt in range(NT0 if qt == 0 else NT1):
            o = half * 1024 + qt * 512 - kt * 128
            dist = (o + j - i).astype(np.float32)
            dt[:, idx, :] = np.where(dist >= 0, -dist, NEGBIG)
            idx += 1
    p = np.arange(128)[:, None, None]
    q8 = np.arange(8)[None, :, None]
    n = np.arange(8)[None, None, :]
    pos = half * 1024 + q8 * 128 + p
    own = pos // 256
    pm = np.where(n < own, 0.0, -1.0e30).astype(np.float32).reshape(128, 64)
    oi = (n == own).astype(np.float32).reshape(128, 64)
    esel = np.zeros((8, 8, 128), np.float32)
    for r in range(8):
        esel[r, r, :] = 1.0
    return {
        "dtab": np.ascontiguousarray(dt.reshape(128, -1)),
        "pmask": pm, "ownind": oi,
        "ident": np.eye(128, dtype=np.float32).astype(bf),
        "onesb": np.ones((128, 128), np.float32).astype(bf),
        "onesf": np.ones((128, 128), np.float32),
        "esel": esel.reshape(8, 1024).astype(bf),
    }


_NC_CACHE = {}
_WSHAPE = {}


def _get_nc(n_layers=DEPTH, debug=False):
    key = (n_layers, debug)
    if key not in _NC_CACHE:
        b = Builder(n_layers, debug)
        _NC_CACHE[key] = b.build()
        _WSHAPE[key] = b.wshape
    return _NC_CACHE[key]


def _in_maps(inputs, wshape):
    x = np.asarray(inputs["x"], np.float32)
    f = lambda k: np.ascontiguousarray(np.asarray(inputs[k], np.float32))
    gl = []
    for name in ["g_pre_mix", "g_post_mix", "g_pre_mlp", "g_post_mlp"]:
        g = np.asarray(inputs[name], np.float32).reshape(DEPTH, NC_, 128)
        gl.append(g)
    gains = np.stack(gl, axis=1)
    gains = np.ascontiguousarray(gains.transpose(3, 0, 1, 2).reshape(128, -1))
    gsub = np.asarray(inputs["g_subln"], np.float32).reshape(DEPTH, 2, 128)
    gsub = np.ascontiguousarray(gsub.transpose(2, 0, 1).reshape(128, -1))
    lv = np.stack([np.asarray(inputs[k], np.float32) for k in ["lam_q1", "lam_k1", "lam_q2", "lam_k2"]], axis=1)
    lamv = np.ascontiguousarray(lv.transpose(2, 0, 1).reshape(128, -1))
    shared = {"gains": gains, "gsub": gsub, "lamv": lamv}
    for l in range(DEPTH):
        for nm in ["w_in", "w_proj_a", "w_proj_b", "w_out", "w_up", "w_down"]:
            shp = wshape[(nm, l)]
            w = np.asarray(inputs[nm][l], np.float32)
            shared[f"{nm}{l}"] = np.ascontiguousarray(w) if list(w.shape) == list(shp) else np.zeros(shp, np.float32)
    cs = [_consts(0), _consts(1)]
    maps = []
    for c in range(8):
        b, half = c // 2, c % 2
        xs = x[b, half * T:(half + 1) * T, :]
        xT = np.ascontiguousarray(xs.T).reshape(NC_, 128, T)
        m = {"xT": xT}
        m.update(shared)

        m.update(cs[half])
        maps.append(m)
    return maps


def kernel(**inputs):
    nc = _get_nc()
    maps = _in_maps(inputs, _WSHAPE[(DEPTH, False)])
    res = run_bass_kernel_spmd(nc, maps, core_ids=list(range(8)))
    out = np.empty((4, 2048, D), np.float32)
    for c in range(8):
        b, half = c // 2, c % 2
        oT = np.asarray(res.results[c]["outT"]).reshape(D, T)
        out[b, half * T:(half + 1) * T, :] = oT.T
    return out
```
